# Optimizing a Trainium2 kernel written in Bass

```python
import jax
import jax.numpy as jnp
from jax import lax
import numpy as np

D_MODEL = 4096
BATCH = 1
SEQ = 16384
DEPTH = 2

GRID_W = 64
CTX_LEN = 256
MIX_W = D_MODEL
HEAD_DIM = 128
N_Q_HEADS = (MIX_W // 2) // HEAD_DIM
N_KV_HEADS = N_Q_HEADS // 4
Q_PER_KV = N_Q_HEADS // N_KV_HEADS
ATTN_W = N_Q_HEADS * HEAD_DIM
KV_W = N_KV_HEADS * HEAD_DIM
WINDOW = 128
ATTN_BLOCK = 128
AXIS_ROT = HEAD_DIM // 2
ROPE_BASE = 10000.0
CONV_W = MIX_W // 4
CONV_K = 3
FOURIER_W = MIX_W // 4
FOURIER_GROUPS = 4
FOURIER_GROUP_W = FOURIER_W // FOURIER_GROUPS
IN_W = ATTN_W + 2 * KV_W + 3 * CONV_W + FOURIER_W
SPLITS = [ATTN_W, ATTN_W + KV_W, ATTN_W + 2 * KV_W, ATTN_W + 2 * KV_W + CONV_W,
          ATTN_W + 2 * KV_W + 2 * CONV_W, ATTN_W + 2 * KV_W + 3 * CONV_W]
N_EXPERTS = 32
TOP_K = 4
D_EXPERT = 768
SWIGLU_LIMIT = 7.0
SWIGLU_ALPHA = 1.702
MOE_BLOCK = 128
N_MOD = 6
RMS_EPS = 1e-5
NEG_INF = -1e30

kernel_name = 'hybrid_swa_shortconv_fnet_moe_dit'


def rmsnorm(x, g):
    xf = x.astype(jnp.float32)
    y = xf * lax.rsqrt(jnp.mean(xf * xf, axis=-1, keepdims=True) + RMS_EPS)
    return (y * g.astype(jnp.float32)).astype(x.dtype)


def adaln_params(cond, w_ada, b_ada):
    m = jax.nn.silu(cond) @ w_ada + b_ada
    return m.reshape(cond.shape[0], N_MOD, D_MODEL)


def modulate(h, shift, scale):
    return h * (1.0 + scale[:, None, :]) + shift[:, None, :]


def axial_rope_angles(rows):
    row = jnp.repeat(jnp.arange(rows, dtype=jnp.float32), GRID_W)
    col = jnp.tile(jnp.arange(GRID_W, dtype=jnp.float32), rows)
    inv_freq = ROPE_BASE ** (-jnp.arange(0, AXIS_ROT, 2, dtype=jnp.float32) / AXIS_ROT)
    ang = jnp.concatenate([row[:, None] * inv_freq, col[:, None] * inv_freq], axis=-1)
    return jnp.cos(ang), jnp.sin(ang)


def apply_axial_rope(x, cos, sin):
    b, t, h, _ = x.shape
    half = AXIS_ROT // 2
    xf = x.astype(jnp.float32).reshape(b, t, h, 2, 2, half)
    x1, x2 = xf[..., 0, :], xf[..., 1, :]
    cs = cos.reshape(t, 1, 2, half)
    sn = sin.reshape(t, 1, 2, half)
    out = jnp.stack([x1 * cs - x2 * sn, x1 * sn + x2 * cs], axis=-2)
    return out.reshape(b, t, h, HEAD_DIM).astype(x.dtype)


def band_mask(n_blocks, n_tok):
    blk = jnp.arange(n_blocks)[:, None, None]
    qpos = blk * ATTN_BLOCK + jnp.arange(ATTN_BLOCK)[None, :, None]
    kpos = (blk - 1) * ATTN_BLOCK + jnp.arange(3 * ATTN_BLOCK)[None, None, :]
    return (jnp.abs(kpos - qpos) <= WINDOW) & (kpos >= 0) & (kpos < n_tok)


def sink_column(attn_sink, lead_shape, n_q):
    s = attn_sink.astype(jnp.float32).reshape(N_KV_HEADS, Q_PER_KV, 1, 1)
    return jnp.broadcast_to(s, lead_shape + (N_KV_HEADS, Q_PER_KV, n_q, 1))


def windowed_attention(q, k, v, kc, vc, attn_sink):
    b, t = q.shape[:2]
    nb = t // ATTN_BLOCK
    scale = HEAD_DIM ** -0.5
    qb = q.reshape(b, nb, ATTN_BLOCK, N_KV_HEADS, Q_PER_KV, HEAD_DIM)

    def band(a):
        ap = jnp.pad(a, ((0, 0), (ATTN_BLOCK, ATTN_BLOCK), (0, 0), (0, 0)))
        ap = ap.reshape(b, nb + 2, ATTN_BLOCK, N_KV_HEADS, HEAD_DIM)
        return jnp.concatenate([ap[:, :-2], ap[:, 1:-1], ap[:, 2:]], axis=2)

    kb, vb = band(k), band(v)
    s_loc = jnp.einsum('bnqhgd,bnkhd->bnhgqk', qb, kb).astype(jnp.float32) * scale
    mask = band_mask(nb, t)[None, :, None, None]
    s_loc = jnp.where(mask, s_loc, NEG_INF)
    s_ctx = jnp.einsum('bnqhgd,bchd->bnhgqc', qb, kc).astype(jnp.float32) * scale
    logits = jnp.concatenate([s_loc, s_ctx, sink_column(attn_sink, (b, nb), ATTN_BLOCK)], axis=-1)
    p = jax.nn.softmax(logits, axis=-1)
    n_loc = 3 * ATTN_BLOCK
    n_ctx = kc.shape[1]
    p_loc = p[..., :n_loc].astype(v.dtype)
    p_ctx = p[..., n_loc:n_loc + n_ctx].astype(v.dtype)
    o = (jnp.einsum('bnhgqk,bnkhd->bnqhgd', p_loc, vb)
         + jnp.einsum('bnhgqc,bchd->bnqhgd', p_ctx, vc))
    return o.reshape(b, t, ATTN_W)


def context_attention(qc, kc, vc, attn_sink):
    b, lc = qc.shape[:2]
    scale = HEAD_DIM ** -0.5
    qg = qc.reshape(b, lc, N_KV_HEADS, Q_PER_KV, HEAD_DIM)
    s = jnp.einsum('bqhgd,bkhd->bhgqk', qg, kc).astype(jnp.float32) * scale
    logits = jnp.concatenate([s, sink_column(attn_sink, (b,), lc)], axis=-1)
    p = jax.nn.softmax(logits, axis=-1)[..., :lc].astype(vc.dtype)
    o = jnp.einsum('bhgqk,bkhd->bqhgd', p, vc)
    return o.reshape(b, lc, ATTN_W)


def short_conv_gate(bg, cg, hv, conv_w):
    t = hv.shape[1]
    z = cg * hv
    zp = jnp.pad(z, ((0, 0), (CONV_K // 2, CONV_K // 2), (0, 0)))
    y = zp[:, 0:t] * conv_w[0]
    for j in range(1, CONV_K):
        y = y + zp[:, j:j + t] * conv_w[j]
    return bg * y


def fourier_mix(u):
    b, t, _ = u.shape
    ug = u.astype(jnp.float32).reshape(b, t, FOURIER_GROUPS, FOURIER_GROUP_W)
    f = jnp.fft.fftn(ug, axes=(1, 3), norm='ortho').real
    return f.reshape(b, t, FOURIER_W).astype(u.dtype)


def moe_ffn(h, w_router, b_router, w_gu, b_gu, w_dn, b_dn):
    n_tok, d = h.shape
    logits = h.astype(jnp.float32) @ w_router.astype(jnp.float32) + b_router.astype(jnp.float32)
    top_val, top_idx = lax.top_k(logits, TOP_K)
    gate_w = jax.nn.softmax(top_val, axis=-1)
    n_asg = n_tok * TOP_K
    flat_e = top_idx.reshape(-1).astype(jnp.int32)
    flat_tok = jnp.repeat(jnp.arange(n_tok, dtype=jnp.int32), TOP_K)
    flat_w = gate_w.reshape(-1)
    order = jnp.argsort(flat_e)
    e_sorted = flat_e[order]
    counts = jnp.bincount(flat_e, length=N_EXPERTS).astype(jnp.int32)
    padded = (counts + MOE_BLOCK - 1) // MOE_BLOCK * MOE_BLOCK
    cum_pad = jnp.cumsum(padded)
    pad_start = cum_pad - padded
    start = jnp.cumsum(counts) - counts
    dest = pad_start[e_sorted] + jnp.arange(n_asg, dtype=jnp.int32) - start[e_sorted]
    n_blocks = -(-n_asg // MOE_BLOCK) + N_EXPERTS
    n_slots = n_blocks * MOE_BLOCK
    slot_tok = jnp.full((n_slots,), n_tok, jnp.int32).at[dest].set(flat_tok[order])
    slot_w = jnp.zeros((n_slots,), jnp.float32).at[dest].set(flat_w[order])
    block_start = jnp.arange(n_blocks, dtype=jnp.int32) * MOE_BLOCK
    block_e = jnp.minimum(jnp.searchsorted(cum_pad, block_start, side='right'), N_EXPERTS - 1)
    h_pad = jnp.concatenate([h, jnp.zeros((1, d), h.dtype)], axis=0)

    def expert_block(args):
        tok, e, w = args
        xb = h_pad[tok]
        gu = xb @ w_gu[e] + b_gu[e]
        gate = jnp.minimum(gu[:, :D_EXPERT], SWIGLU_LIMIT)
        up = jnp.clip(gu[:, D_EXPERT:], -SWIGLU_LIMIT, SWIGLU_LIMIT)
        act = (up + 1.0) * (gate * jax.nn.sigmoid(SWIGLU_ALPHA * gate))
        yb = act @ w_dn[e] + b_dn[e]
        return yb.astype(jnp.float32) * w[:, None]

    ys = lax.map(expert_block, (slot_tok.reshape(n_blocks, MOE_BLOCK), block_e,
                                slot_w.reshape(n_blocks, MOE_BLOCK)))
    out = jax.ops.segment_sum(ys.reshape(n_slots, d), slot_tok, num_segments=n_tok + 1)[:n_tok]
    return out.astype(h.dtype)


def hybrid_layer(x, xc, c, c_ctx, w_ada, b_ada, g_mix, w_in, conv_w, attn_sink, w_out,
                 g_ffn, w_router, b_router, w_gu, b_gu, w_dn, b_dn, rope_cos, rope_sin, last):
    b, t, d = x.shape
    lc = xc.shape[1]
    mod = adaln_params(c, w_ada, b_ada)
    mod_c = adaln_params(c_ctx[None], w_ada, b_ada)
    h = modulate(rmsnorm(x, g_mix), mod[:, 0], mod[:, 1])
    hc = modulate(rmsnorm(xc, g_mix), mod_c[:, 0], mod_c[:, 1])

    q, k, v, bg, cg, hv, fu = jnp.split(h @ w_in, SPLITS, axis=-1)
    q = apply_axial_rope(q.reshape(b, t, N_Q_HEADS, HEAD_DIM), rope_cos, rope_sin)
    k = apply_axial_rope(k.reshape(b, t, N_KV_HEADS, HEAD_DIM), rope_cos, rope_sin)
    v = v.reshape(b, t, N_KV_HEADS, HEAD_DIM)

    if last:
        kvc = hc @ w_in[:, ATTN_W:ATTN_W + 2 * KV_W]
        kc, vc = jnp.split(kvc, [KV_W], axis=-1)
    else:
        qc, kc, vc, bgc, cgc, hvc, fuc = jnp.split(hc @ w_in, SPLITS, axis=-1)
    kc = kc.reshape(b, lc, N_KV_HEADS, HEAD_DIM)
    vc = vc.reshape(b, lc, N_KV_HEADS, HEAD_DIM)

    mixed = jnp.concatenate([windowed_attention(q, k, v, kc, vc, attn_sink),
                             short_conv_gate(bg, cg, hv, conv_w),
                             fourier_mix(fu)], axis=-1) @ w_out
    x = x + mod[:, 2][:, None, :] * mixed
    h2 = modulate(rmsnorm(x, g_ffn), mod[:, 3], mod[:, 4])

    if last:
        y = moe_ffn(h2.reshape(b * t, d), w_router, b_router, w_gu, b_gu, w_dn, b_dn)
        x = x + mod[:, 5][:, None, :] * y.reshape(b, t, d)
        return x, None

    mixed_c = jnp.concatenate([context_attention(qc.reshape(b, lc, N_Q_HEADS, HEAD_DIM), kc, vc, attn_sink),
                               short_conv_gate(bgc, cgc, hvc, conv_w),
                               fourier_mix(fuc)], axis=-1) @ w_out
    xc = xc + mod_c[:, 2][:, None, :] * mixed_c
    h2c = modulate(rmsnorm(xc, g_ffn), mod_c[:, 3], mod_c[:, 4])
    tokens = jnp.concatenate([h2.reshape(b * t, d), h2c.reshape(b * lc, d)], axis=0)
    y = moe_ffn(tokens, w_router, b_router, w_gu, b_gu, w_dn, b_dn)
    x = x + mod[:, 5][:, None, :] * y[:b * t].reshape(b, t, d)
    xc = xc + mod_c[:, 5][:, None, :] * y[b * t:].reshape(b, lc, d)
    return x, xc


def setup_inputs(seed: int = 0) -> dict:
    key = jax.random.key(seed)
    ks = jax.random.split(key, 20)

    def nrm(k, shape, s):
        return jax.random.normal(k, shape, jnp.float32) * s

    L = DEPTH
    return {
        'x': nrm(ks[0], (BATCH, SEQ, D_MODEL), 1.0),
        'c': nrm(ks[1], (BATCH, D_MODEL), 1.0),
        'ctx': nrm(ks[2], (BATCH, CTX_LEN, D_MODEL), 1.0),
        'c_ctx': nrm(ks[3], (D_MODEL,), 1.0),
        'w_ada': nrm(ks[4], (L, D_MODEL, N_MOD * D_MODEL), 0.5 * D_MODEL ** -0.5),
        'b_ada': nrm(ks[5], (L, N_MOD * D_MODEL), 0.02),
        'g_mix': 1.0 + nrm(ks[6], (L, D_MODEL), 0.05),
        'w_in': nrm(ks[7], (L, D_MODEL, IN_W), D_MODEL ** -0.5),
        'conv_w': nrm(ks[8], (L, CONV_K, CONV_W), CONV_K ** -0.5),
        'attn_sink': nrm(ks[9], (L, N_Q_HEADS), 0.5),
        'w_out': nrm(ks[10], (L, MIX_W, D_MODEL), MIX_W ** -0.5),
        'g_ffn': 1.0 + nrm(ks[11], (L, D_MODEL), 0.05),
        'w_router': nrm(ks[12], (L, D_MODEL, N_EXPERTS), D_MODEL ** -0.5),
        'b_router': nrm(ks[13], (L, N_EXPERTS), 0.01),
        'w_gu': nrm(ks[14], (L, N_EXPERTS, D_MODEL, 2 * D_EXPERT), D_MODEL ** -0.5),
        'b_gu': nrm(ks[15], (L, N_EXPERTS, 2 * D_EXPERT), 0.02),
        'w_dn': nrm(ks[16], (L, N_EXPERTS, D_EXPERT, D_MODEL), D_EXPERT ** -0.5),
        'b_dn': nrm(ks[17], (L, N_EXPERTS, D_MODEL), 0.02),
        'g_final': 1.0 + nrm(ks[18], (D_MODEL,), 0.05),
    }


def reference(x, c, ctx, c_ctx, w_ada, b_ada, g_mix, w_in, conv_w, attn_sink, w_out, g_ffn,
              w_router, b_router, w_gu, b_gu, w_dn, b_dn, g_final):
    rows = x.shape[1] // GRID_W
    rope_cos, rope_sin = axial_rope_angles(rows)
    xc = ctx
    for l in range(DEPTH):
        x, xc = hybrid_layer(x, xc, c, c_ctx, w_ada[l], b_ada[l], g_mix[l], w_in[l], conv_w[l],
                             attn_sink[l], w_out[l], g_ffn[l], w_router[l], b_router[l],
                             w_gu[l], b_gu[l], w_dn[l], b_dn[l], rope_cos, rope_sin,
                             l == DEPTH - 1)
    return rmsnorm(x, g_final)
```

```python
import numpy as np
import ml_dtypes
import concourse.bass as bass
import concourse.mybir as mybir
from concourse.bass_utils import run_bass_kernel_spmd

F32 = mybir.dt.float32
BF16 = mybir.dt.bfloat16
AF = mybir.ActivationFunctionType
ALU = mybir.AluOpType
NPBF = ml_dtypes.bfloat16

NCORES = 8
D = 4096
SEQ = 16384
TL = SEQ // NCORES
NEXT = TL + 256
LC = 256
INW = 7168
NE = 32
DE = 768
EPS = 1e-5


class Sched:
    ENGS = ("pe", "act", "dve", "pool", "sp")
    NDMASEM = 8

    def __init__(self, nc):
        self.nc = nc
        self.cnt = {e: 0 for e in self.ENGS}
        self.known = {e: {} for e in self.ENGS}
        self.prog = {e: [] for e in self.ENGS}
        self.snap = {}
        self.dval = {}
        self.drr = {}
        self.lastw = {}
        self.readers = {}

    def _learn(self, e, ev):
        k = self.known[e]
        if ev[0] == "c":
            sn = self.snap.get((ev[1], ev[2]))
            key, val = ("c", ev[1]), ev[2]
        else:
            sn = ev[3]
            key, val = ("d", ev[1]), ev[2]
        if k.get(key, 0) < val:
            k[key] = val
        if sn:
            for kk, vv in sn.items():
                if k.get(kk, 0) < vv:
                    k[kk] = vv

    def _deps(self, reads, writes):
        evs = []
        for r in reads:
            w = self.lastw.get(r)
            if w is not None:
                evs.append(w)
        for r in writes:
            w = self.lastw.get(r)
            if w is not None:
                evs.append(w)
            evs.extend(self.readers.get(r, ()))
        return evs

    def _emit_waits(self, e, evs):
        waits = {}
        for ev in evs:
            if ev[0] == "c":
                key, val = ("c", ev[1]), ev[2]
            else:
                key, val = ("d", ev[1]), ev[2]
            if self.known[e].get(key, 0) >= val:
                continue
            if waits.get(key, (0, None))[0] < val:
                waits[key] = (val, ev)
        for key, (val, ev) in waits.items():
            if self.known[e].get(key, 0) >= val:
                continue
            self.prog[e].append(("w", key, val))
            self._learn(e, ev)

    def _record(self, ev, reads, writes):
        for r in writes:
            self.lastw[r] = ev
            self.readers[r] = []
        for r in reads:
            if r not in writes:
                self.readers.setdefault(r, []).append(ev)

    def op(self, e, fn, reads=(), writes=()):
        self._emit_waits(e, self._deps(reads, writes))
        self.cnt[e] += 1
        idx = self.cnt[e]
        self.prog[e].append(("o", fn, idx))
        if e == "pe":
            self.known[e][("c", e)] = idx
        self.snap[(e, idx)] = dict(self.known[e])
        ev = ("c", e, idx)
        self._record(ev, reads, writes)
        return ev

    def dma(self, q, fn, reads=(), writes=()):
        slot = self.drr.get(q, 0) % self.NDMASEM
        self.drr[q] = self.drr.get(q, 0) + 1
        skey = f"{q}{slot}"
        prev = self.dval.get(skey, 0)
        evs = self._deps(reads, writes)
        if prev:
            evs.append(("d", skey, prev, None))
        self._emit_waits(q, evs)
        val = prev + 16
        self.dval[skey] = val
        self.prog[q].append(("d", fn, skey))
        ev = ("d", skey, val, dict(self.known[q]))
        self._record(ev, reads, writes)
        return ev

    def finish(self):
        evs = [("c", f, self.cnt[f]) for f in ("pe", "act", "dve", "pool") if self.cnt[f]]
        for skey, v in self.dval.items():
            evs.append(("d", skey, v, None))
        self._emit_waits("sp", evs)

    def emit(self):
        import contextlib
        nc = self.nc
        self.finish()
        with contextlib.ExitStack() as st:
            semh = {}
            for e in ("pe", "act", "dve", "pool"):
                semh[("c", e)] = st.enter_context(nc.semaphore(f"c_{e}"))
            for skey in self.dval:
                semh[("d", skey)] = st.enter_context(nc.semaphore(f"d_{skey}"))
            st.enter_context(nc.allow_non_contiguous_dma(reason="small strided pieces are intentional"))
            block = st.enter_context(nc.Block())

            def run(e):
                def body(engine):
                    for it in self.prog[e]:
                        if it[0] == "w":
                            engine.wait_ge(semh[it[1]], it[2])
                        elif it[0] == "o":
                            it[1](engine).then_inc(semh[("c", e)], 1)
                        else:
                            it[1](engine).then_inc(semh[("d", it[2])], 16)
                return body
            block.tensor(run("pe"))
            block.scalar(run("act"))
            block.vector(run("dve"))
            block.gpsimd(run("pool"))
            block.sync(run("sp"))


class Ctx:
    def __init__(self):
        self.nc = bass.Bass("TRN2", target_bir_lowering=False)
        self.S = Sched(self.nc)
        self.rr = 0

    def din(self, name, shape, dt=F32):
        return self.nc.dram_tensor(name, list(shape), dt, kind="ExternalInput").ap()

    def dout(self, name, shape, dt=F32):
        return self.nc.dram_tensor(name, list(shape), dt, kind="ExternalOutput").ap()

    def dscr(self, name, shape, dt=F32):
        return self.nc.dram_tensor(name, list(shape), dt).ap()

    def sb(self, name, shape, dt=F32):
        return self.nc.alloc_sbuf_tensor("sb_" + name, list(shape), dt)

    def ps(self, name, shape, dt=F32):
        return self.nc.alloc_psum_tensor("ps_" + name, list(shape), dt)

    def load(self, out, in_, w, r=(), q="sp"):
        return self.S.dma(q, lambda e: e.dma_start(out=out, in_=in_), reads=list(r), writes=list(w))

    def store(self, out, in_, r, w=(), q="pool"):
        return self.S.dma(q, lambda e: e.dma_start(out=out, in_=in_), reads=list(r), writes=list(w))

    def mm(self, out, lhsT, rhs, start, stop, r, w):
        return self.S.op("pe", lambda e: e.matmul(out, lhsT=lhsT, rhs=rhs, start=start, stop=stop),
                         reads=list(r), writes=list(w))

    def act(self, out, in_, func, r, w, bias=None, scale=None):
        kw = {}
        if bias is not None:
            kw["bias"] = bias
        if scale is not None:
            kw["scale"] = scale
        return self.S.op("act", lambda e: e.activation(out=out, in_=in_, func=func, **kw),
                         reads=list(r), writes=list(w))

    def tt(self, eng, out, in0, in1, op, r, w):
        return self.S.op(eng, lambda e: e.tensor_tensor(out=out, in0=in0, in1=in1, op=op),
                         reads=list(r), writes=list(w))

    def ts(self, eng, out, in0, s1, op0, r, w, s2=None, op1=None):
        if op1 is None:
            return self.S.op(eng, lambda e: e.tensor_scalar(out=out, in0=in0, scalar1=s1, scalar2=None, op0=op0),
                             reads=list(r), writes=list(w))
        return self.S.op(eng, lambda e: e.tensor_scalar(out=out, in0=in0, scalar1=s1, scalar2=s2, op0=op0, op1=op1),
                         reads=list(r), writes=list(w))

    def stt(self, eng, out, in0, scalar, in1, op0, op1, r, w):
        return self.S.op(eng, lambda e: e.scalar_tensor_tensor(out=out, in0=in0, scalar=scalar, in1=in1, op0=op0, op1=op1),
                         reads=list(r), writes=list(w))

    def copy(self, eng, out, in_, r, w):
        if eng == "act":
            return self.act(out, in_, AF.Copy, r, w)
        return self.S.op(eng, lambda e: e.tensor_copy(out=out, in_=in_), reads=list(r), writes=list(w))

    def anyeng(self, choices=("dve", "pool", "act")):
        self.rr += 1
        return choices[self.rr % len(choices)]


def fm(ap):
    return ap.rearrange("(c p) t -> p c t", p=128)


NA = 24576 // NCORES


def build_L0():
    C = Ctx()
    S = C.S
    cond = C.din("cond", [128, 32, 2])
    wada = C.din("wada", [2, D, NA])
    bada = C.din("bada", [2, 2, NA])
    mod = C.dout("mod", [2, 2, NA])
    sc = C.sb("sc", [128, 32, 2])
    bt = C.sb("bt", [2, 2, NA])
    res = C.sb("res", [2, 2, NA])
    wt = [C.sb(f"wt{i}", [128, NA]) for i in range(3)]
    pss = [C.ps(f"ps{i}", [2, 512]) for i in range(6)]
    C.load(sc[:], cond, ["sc"])
    C.load(bt[:], bada.rearrange("l i n -> i l n"), ["bt"])
    C.act(sc[:], sc[:], AF.Silu, ["sc"], ["sc"])
    for l in range(2):
        for kc in range(32):
            s = (l * 32 + kc) % 3
            C.load(wt[s][:], wada[l, kc * 128:(kc + 1) * 128, :], [("wt", s)])
            for g in range(6):
                C.mm(pss[g][:], sc[:, kc, :], wt[s][:, g * 512:(g + 1) * 512], kc == 0, kc == 31,
                     ["sc", ("wt", s)], [("ps", g)])
        for g in range(6):
            C.tt("dve", res[:, l, g * 512:(g + 1) * 512], pss[g][:], bt[:, l, g * 512:(g + 1) * 512], ALU.add,
                 [("ps", g), "bt"], ["res"])
    C.store(mod.rearrange("l i n -> i l n"), res[:], ["res"], ["mod"], q="sp")
    S.emit()
    return C.nc


def convert_weights(C, src, dst, rows, cols, xs, wt, piece=3584):
    it = 0
    for kc in range(rows // 128):
        for c0 in range(0, cols, piece):
            w = min(piece, cols - c0)
            s = it % 2
            it += 1
            xf = xs[s][:].rearrange("p c t -> p (c t)")[:, 0:w]
            wf = wt[s][:].rearrange("p c t -> p (c t)")[:, 0:w]
            C.load(xf, src[kc * 128:(kc + 1) * 128, c0:c0 + w], [("xs", s)])
            C.copy(C.anyeng(), wf, xf, [("xs", s)], [("wt", s)])
            C.store(dst[kc * 128:(kc + 1) * 128, c0:c0 + w], wf, [("wt", s)], ["wbf"])


def rms_modulate(C, src_fm, t0, T, xs, hT, sq, ssq, rstd, tmp, ones, gs, shift, h32=None):
    for cgi in range(4):
        s = cgi % 2
        C.load(xs[s][:, :, 0:T], src_fm[:, cgi * 8:(cgi + 1) * 8, t0:t0 + T], [("xs", s)])
        for c in range(8):
            q = c % 2
            C.act(sq[q][:, 0:T], xs[s][:, c, 0:T], AF.Square, [("xs", s)], [("sq", q)])
            C.mm(ssq[:, 0:T], ones[:], sq[q][:, 0:T], cgi == 0 and c == 0, cgi == 3 and c == 7,
                 [("sq", q), "ones"], ["ssq"])
    C.ts("dve", rstd[:, 0:T], ssq[:, 0:T], 1.0 / D, ALU.mult, ["ssq"], ["rstd"], s2=EPS, op1=ALU.add)
    C.act(rstd[:, 0:T], rstd[:, 0:T], AF.Sqrt, ["rstd"], ["rstd"])
    C.S.op("dve", lambda e, T=T: e.reciprocal(out=rstd[:, 0:T], in_=rstd[:, 0:T]), reads=["rstd"], writes=["rstd"])
    for cgi in range(4):
        s = cgi % 2
        C.load(xs[s][:, :, 0:T], src_fm[:, cgi * 8:(cgi + 1) * 8, t0:t0 + T], [("xs", s)])
        for c in range(8):
            cc = cgi * 8 + c
            q = c % 2
            C.tt("dve", tmp[q][:, 0:T], xs[s][:, c, 0:T], rstd[:, 0:T], ALU.mult, [("xs", s), "rstd"], [("tmp", q)])
            C.act(hT[:, cc, 0:T], tmp[q][:, 0:T], AF.Identity, [("tmp", q), "vec"], ["hT"],
                  bias=shift[:, cc:cc + 1], scale=gs[:, cc:cc + 1])


def build_L1(last, dbg=False):
    C = Ctx()
    S = C.S
    if dbg:
        dbg_h = C.dout("dbg_h", [128, 32, 256], BF16)
        dbg_r = C.dout("dbg_r", [128, 256])
        dbg_w = C.dout("dbg_w", [128, 32, 512], BF16)
    xT = C.din("xT", [D, NEXT])
    xcT = C.din("xcT", [D, LC])
    vecd = C.din("vec", [128, 192])
    w_in = C.din("w_in", [D, INW])
    ropec = C.din("ropec", [128, NEXT])
    ropes = C.din("ropes", [128, NEXT])
    permd = C.din("perm", [128, 128])
    dftd = C.din("dft", [128, 2, 3, 256], BF16)
    qT = C.dout("qT", [2048, TL], BF16)
    kT = C.dout("kT", [512, NEXT], BF16)
    vO = C.dout("v", [NEXT, 512], BF16)
    kcT = C.dout("kcT", [512, LC], BF16)
    vcO = C.dout("vc", [LC, 512], BF16)
    convT = C.dout("convT", [1024, TL], BF16)
    AB = C.dout("AB", [2, TL, 1024], BF16)
    if not last:
        qcT = C.dout("qcT", [2048, LC], BF16)
        convcT = C.dout("convcT", [1024, LC], BF16)
        fourcT = C.dout("fourcT", [1024, LC], BF16)
    wbf = C.dscr("wbf", [D, INW], BF16)
    Z = C.dscr("Z", [1024, NEXT])
    BG = C.dscr("BG", [1024, NEXT], BF16)
    Zc = C.dscr("Zc", [1024, LC + 2])
    BGc = C.dscr("BGc", [1024, LC], BF16)

    xs = [C.sb(f"xs{i}", [128, 8, 512]) for i in range(2)]
    wt = [C.sb(f"wt{i}", [128, 32, 512], BF16) for i in range(2)]
    hT = C.sb("hT", [128, 32, 512], BF16)
    cgs = C.sb("cgs", [128, 8, 512])
    fuT = C.sb("fuT", [128, 8, 512], BF16)
    vec = C.sb("vecs", [128, 192])
    gsL = C.sb("gsL", [128, 32])
    gsC = C.sb("gsC", [128, 32])
    ones = C.sb("ones", [128, 128], BF16)
    perm = C.sb("perm", [128, 128])
    dft = C.sb("dfts", [128, 2, 3, 256], BF16)
    sq = [C.sb(f"sq{i}", [128, 512], BF16) for i in range(2)]
    tmp = [C.sb(f"tmp{i}", [128, 512]) for i in range(2)]
    rstd = C.sb("rstd", [128, 512])
    rc = C.sb("rc", [128, 512])
    rs = C.sb("rs", [128, 512])
    xq = [C.sb(f"xq{i}", [128, 512]) for i in range(2)]
    st16 = [C.sb(f"st16_{i}", [128, 512], BF16) for i in range(3)]
    st32 = [C.sb(f"st32_{i}", [128, 514]) for i in range(2)]
    zero = C.sb("zero", [128, 2])
    abc = C.sb("abc", [128, 2, 2, 1024], BF16)
    ssq = C.ps("ssq", [128, 512])
    pacc = [C.ps(f"pacc{i}", [128, 512]) for i in range(3)]
    pperm = [C.ps(f"pperm{i}", [128, 512]) for i in range(2)]

    C.load(vec[:], vecd, ["vec"])
    C.load(perm[:], permd, ["perm"])
    C.load(dft[:], dftd, ["dft"])
    S.op("pool", lambda e: e.memset(ones[:], 1.0), writes=["ones"])
    S.op("pool", lambda e: e.memset(zero[:], 0.0), writes=["zero"])
    C.ts("dve", gsL[:], vec[:, 64:96], 1.0, ALU.add, ["vec"], ["vec"])
    C.tt("dve", gsL[:], gsL[:], vec[:, 0:32], ALU.mult, ["vec"], ["vec"])
    C.ts("dve", gsC[:], vec[:, 128:160], 1.0, ALU.add, ["vec"], ["vec"])
    C.tt("dve", gsC[:], gsC[:], vec[:, 0:32], ALU.mult, ["vec"], ["vec"])
    convert_weights(C, w_in, wbf, D, INW, xs, wt)
    wbf_fm = fm(wbf)
    C.store(Zc[:, 0:1].rearrange("(c p) t -> p c t", p=128), zero[:, 0:1].unsqueeze(1).to_broadcast([128, 8, 1]), ["zero"], ["Zc"])
    C.store(Zc[:, LC + 1:LC + 2].rearrange("(c p) t -> p c t", p=128), zero[:, 0:1].unsqueeze(1).to_broadcast([128, 8, 1]), ["zero"], ["Zc"])

    tiles = [("HL", 0, 128, "halo"), ("L0", 128, 512, "lat"), ("L1", 640, 512, "lat"),
             ("L2", 1152, 512, "lat"), ("L3", 1664, 512, "lat"), ("HR", 2176, 128, "halo"), ("C", 0, LC, "ctx")]
    wl = 0
    pa = 0
    s16 = 0
    for name, t0, T, kind in tiles:
        isctx = kind == "ctx"
        src = fm(xcT) if isctx else fm(xT)
        rms_modulate(C, src, t0, T, xs, hT, sq, ssq, rstd, tmp, ones,
                     gsC if isctx else gsL, vec[:, 96:128] if isctx else vec[:, 32:64])
        if dbg and isctx:
            C.store(dbg_h, hT[:, :, 0:256], ["hT"], ["dbgh"])
            C.store(dbg_r, rstd[:, 0:256], ["rstd"], ["dbgr"])
        if kind == "lat" or kind == "halo":
            C.load(rc[:, 0:T], ropec[:, t0:t0 + T], ["rc"])
            C.load(rs[:, 0:T], ropes[:, t0:t0 + T], ["rs"])
        if kind == "lat" or (isctx and not last):
            groups = list(range(14))
        elif kind == "halo":
            groups = [4, 5, 8, 9, 10, 11]
        else:
            groups = [4, 5]
        for g in groups:
            ws = wl % 2
            wl += 1
            C.load(wt[ws][:], wbf_fm[:, :, g * 512:(g + 1) * 512], [("wt", ws)], ["wbf"])
            if dbg and isctx and g == 5:
                C.store(dbg_w, wt[ws][:], [("wt", ws)], ["dbgw"])
            if g == 5:
                for tb in range(T // 128):
                    p = pacc[pa % 3]; pk = ("pacc", pa % 3); pa += 1
                    for kc in range(32):
                        C.mm(p[:], hT[:, kc, tb * 128:(tb + 1) * 128], wt[ws][:, kc, :], kc == 0, kc == 31,
                             ["hT", ("wt", ws)], [pk])
                    sb_ = st16[s16 % 3]; sk = ("st16", s16 % 3); s16 += 1
                    C.copy("act", sb_[:], p[:], [pk], [sk])
                    dst = vcO[tb * 128:(tb + 1) * 128, :] if isctx else vO[t0 + tb * 128:t0 + (tb + 1) * 128, :]
                    C.store(dst, sb_[:], [sk], ["vout"])
                continue
            for j in range(4):
                n = 4 * g + j
                p = pacc[pa % 3]; pk = ("pacc", pa % 3); pa += 1
                for kc in range(32):
                    C.mm(p[:, 0:T], wt[ws][:, kc, j * 128:(j + 1) * 128], hT[:, kc, 0:T], kc == 0, kc == 31,
                         ["hT", ("wt", ws)], [pk])
                if g <= 4:
                    sb_ = st16[s16 % 3]; sk = ("st16", s16 % 3); s16 += 1
                    if isctx:
                        C.copy("act", sb_[:, 0:T], p[:, 0:T], [pk], [sk])
                        dst = qcT[n * 128:(n + 1) * 128, :] if g < 4 else kcT[(n - 16) * 128:(n - 15) * 128, :]
                    else:
                        xi = pa % 2
                        C.copy("act", xq[xi][:, 0:T], p[:, 0:T], [pk], [("xq", xi)])
                        C.mm(pperm[xi][:, 0:T], perm[:], xq[xi][:, 0:T], True, True, ["perm", ("xq", xi)], [("pperm", xi)])
                        C.tt("dve", tmp[xi][:, 0:T], pperm[xi][:, 0:T], rs[:, 0:T], ALU.mult, [("pperm", xi), "rs"], [("tmp", xi)])
                        C.tt("pool", xq[xi][:, 0:T], xq[xi][:, 0:T], rc[:, 0:T], ALU.mult, [("xq", xi), "rc"], [("xq", xi)])
                        C.tt("dve", sb_[:, 0:T], xq[xi][:, 0:T], tmp[xi][:, 0:T], ALU.add, [("xq", xi), ("tmp", xi)], [sk])
                        if g < 4:
                            dst = qT[n * 128:(n + 1) * 128, t0 - 128:t0 - 128 + T]
                        else:
                            dst = kT[(n - 16) * 128:(n - 15) * 128, t0:t0 + T]
                    C.store(dst, sb_[:, 0:T], [sk], ["qkout"])
                elif g in (6, 7):
                    sb_ = st16[s16 % 3]; sk = ("st16", s16 % 3); s16 += 1
                    C.copy("act", sb_[:, 0:T], p[:, 0:T], [pk], [sk])
                    c8 = n - 24
                    dst = BGc[c8 * 128:(c8 + 1) * 128, :] if isctx else BG[c8 * 128:(c8 + 1) * 128, t0:t0 + T]
                    C.store(dst, sb_[:, 0:T], [sk], ["BG"])
                elif g in (8, 9):
                    C.copy("act", cgs[:, n - 32, 0:T], p[:, 0:T], [pk], ["cgs"])
                elif g in (10, 11):
                    c8 = n - 40
                    zi = pa % 2
                    zt = st32[zi]; zk = ("st32", zi)
                    if kind == "halo":
                        fl = vec[:, 184:185] if name == "HL" else vec[:, 185:186]
                        C.stt("dve", zt[:, 0:T], cgs[:, c8, 0:T], fl, p[:, 0:T], ALU.mult, ALU.mult, ["cgs", pk, "vec"], [zk])
                    else:
                        C.tt("dve", zt[:, 0:T], cgs[:, c8, 0:T], p[:, 0:T], ALU.mult, ["cgs", pk], [zk])
                    dst = Zc[c8 * 128:(c8 + 1) * 128, 1:1 + LC] if isctx else Z[c8 * 128:(c8 + 1) * 128, t0:t0 + T]
                    C.store(dst, zt[:, 0:T], [zk], ["Z"])
                else:
                    C.copy("act", fuT[:, n - 48, 0:T], p[:, 0:T], [pk], ["fuT"])
        if kind == "lat" or (isctx and not last):
            for tb in range(T // 128):
                for gi in range(4):
                    p = pacc[pa % 3]; pk = ("pacc", pa % 3); pa += 1
                    for cs in range(2):
                        for kc in range(2):
                            C.mm(p[:, cs * 256:(cs + 1) * 256], fuT[:, gi * 2 + kc, tb * 128:(tb + 1) * 128], dft[:, kc, cs, :],
                                 cs == 0 and kc == 0, cs == 1 and kc == 1, ["fuT", "dft"], [pk])
                    if isctx:
                        C.copy("act", abc[:, tb, :, gi * 256:(gi + 1) * 256], p[:].rearrange("p (a c) -> p a c", a=2), [pk], ["abc"])
                    else:
                        sb_ = st16[s16 % 3]; sk = ("st16", s16 % 3); s16 += 1
                        C.copy("act", sb_[:], p[:], [pk], [sk])
                        tt0 = t0 - 128 + tb * 128
                        C.store(AB[:, tt0:tt0 + 128, gi * 256:(gi + 1) * 256].rearrange("a t c -> t a c"),
                                sb_[:].rearrange("p (a c) -> p a c", a=2), [sk], ["AB"])
            if isctx:
                for c8 in range(8):
                    p = pacc[pa % 3]; pk = ("pacc", pa % 3); pa += 1
                    i = 0
                    for ab, var in ((0, 0), (1, 2)):
                        for tb in range(2):
                            C.mm(p[:, 0:LC], abc[:, tb, ab, c8 * 128:(c8 + 1) * 128], dft[:, tb, var, :], i == 0, i == 3,
                                 ["abc", "dft"], [pk])
                            i += 1
                    sb_ = st16[s16 % 3]; sk = ("st16", s16 % 3); s16 += 1
                    C.act(sb_[:, 0:LC], p[:, 0:LC], AF.Copy, [pk], [sk], scale=1.0 / 256.0)
                    C.store(fourcT[c8 * 128:(c8 + 1) * 128, :], sb_[:, 0:LC], [sk], ["fourc"])
    jobs = [(Z, BG, convT, 128 + 512 * i, 512, 512 * i) for i in range(4)]
    if not last:
        jobs.append((Zc, BGc, convcT, 1, LC, 0))
    k = 0
    for Zs, BGs, dstT, z0, T, o0 in jobs:
        for c8 in range(8):
            zi = k % 2; k += 1
            zt = st32[zi]; zk = ("st32", zi)
            C.load(zt[:, 0:T + 2], Zs[c8 * 128:(c8 + 1) * 128, z0 - 1:z0 + T + 1], [zk], ["Z"])
            bgt = st16[s16 % 3]; bk = ("st16", s16 % 3); s16 += 1
            bo = z0 if Zs is Z else 0
            C.load(bgt[:, 0:T], BGs[c8 * 128:(c8 + 1) * 128, bo:bo + T], [bk], ["BG"])
            y = tmp[zi]; yk = ("tmp", zi)
            C.ts("dve", y[:, 0:T], zt[:, 0:T], vec[:, 160 + c8:161 + c8], ALU.mult, [zk, "vec"], [yk])
            C.stt("dve", y[:, 0:T], zt[:, 1:T + 1], vec[:, 168 + c8:169 + c8], y[:, 0:T], ALU.mult, ALU.add, [zk, "vec", yk], [yk])
            C.stt("dve", y[:, 0:T], zt[:, 2:T + 2], vec[:, 176 + c8:177 + c8], y[:, 0:T], ALU.mult, ALU.add, [zk, "vec", yk], [yk])
            ob = st16[s16 % 3]; ok = ("st16", s16 % 3); s16 += 1
            C.tt("pool", ob[:, 0:T], y[:, 0:T], bgt[:, 0:T], ALU.mult, [yk, bk], [ok])
            C.store(dstT[c8 * 128:(c8 + 1) * 128, o0:o0 + T], ob[:, 0:T], [ok], ["convout"])
    S.emit()
    return C.nc


def fmvec(v):
    v = np.asarray(v, np.float32)
    return np.ascontiguousarray(v.reshape(-1, 128).T)


def rope_tables():
    inv = 10000.0 ** (-np.arange(0, 64, 2, dtype=np.float64) / 64.0)
    t = np.arange(SEQ)
    row, col = (t // 64).astype(np.float64), (t % 64).astype(np.float64)
    d = np.arange(128)
    axis, within = d // 64, d % 64
    ph, f = within // 32, within % 32
    pos = np.where(axis[:, None] == 0, row[None, :], col[None, :])
    ang = (pos * inv[f][:, None]).astype(np.float32).astype(np.float64)
    cos = np.cos(ang).astype(np.float32)
    sin = (np.sin(ang) * np.where(ph == 0, -1.0, 1.0)[:, None]).astype(np.float32)
    perm = np.zeros((128, 128), np.float32)
    partner = np.where(ph == 0, d + 32, d - 32)
    perm[partner, d] = 1.0
    return cos, sin, perm


def dft256_table():
    k = np.arange(256, dtype=np.float64)
    ang = 2 * np.pi * np.outer(k, k) / 256.0
    t = np.stack([np.cos(ang), np.sin(ang), -np.sin(ang)], axis=1)
    return np.ascontiguousarray(t.reshape(2, 128, 3, 256).transpose(1, 0, 2, 3)).astype(NPBF)


def ext_slices(x2d):
    xp = np.zeros((SEQ + 256, x2d.shape[1]), x2d.dtype)
    xp[128:128 + SEQ] = x2d
    return [np.ascontiguousarray(xp[j * TL:j * TL + NEXT].T) for j in range(NCORES)]


def make_vec(j, g, shiftL, scaleL, shiftC, scaleC, conv_w):
    v = np.zeros((128, 192), np.float32)
    v[:, 0:32] = fmvec(g)
    v[:, 32:64] = fmvec(shiftL)
    v[:, 64:96] = fmvec(scaleL)
    v[:, 96:128] = fmvec(shiftC)
    v[:, 128:160] = fmvec(scaleC)
    for jj in range(3):
        v[:, 160 + jj * 8:168 + jj * 8] = fmvec(conv_w[jj])
    v[:, 184] = 0.0 if j == 0 else 1.0
    v[:, 185] = 0.0 if j == NCORES - 1 else 1.0
    return v


def build_L2():
    C = Ctx()
    S = C.S
    Ain = C.din("Ain", [SEQ, 128], BF16)
    Bin = C.din("Bin", [SEQ, 128], BF16)
    Wd = C.din("W", [128, 4, 128], BF16)
    twd = C.din("tw", [128, 2, 128])
    yT = C.dout("yT", [128, SEQ], BF16)
    ZS = C.dscr("ZS", [2, 128, 128, 128], BF16)
    XA = C.sb("XA", [128, SEQ], BF16)
    XB = C.sb("XB", [128, SEQ], BF16)
    ZT = [C.sb(f"ZT{i}", [128, 128, 128], BF16) for i in range(2)]
    W = C.sb("W", [128, 4, 128], BF16)
    tw = C.sb("tw", [128, 2, 128])
    ta = [C.sb(f"ta{i}", [128, 512]) for i in range(2)]
    tb_ = [C.sb(f"tb{i}", [128, 512]) for i in range(2)]
    zo = [[C.sb(f"zo{i}{x}", [128, 512], BF16) for x in range(2)] for i in range(2)]
    pz = [[C.ps(f"pz{i}{x}", [128, 512]) for x in range(2)] for i in range(2)]
    pc = [C.ps(f"pc{i}", [128, 512]) for i in range(2)]
    C.load(W[:], Wd, ["W"])
    C.load(tw[:], twd, ["tw"])
    for h in range(4):
        sl = slice(h * 4096, (h + 1) * 4096)
        C.load(XA[:, sl], Ain.rearrange("(a b) c -> a (b c)", a=128)[:, sl], [("XA", h)])
        C.load(XB[:, sl], Bin.rearrange("(a b) c -> a (b c)", a=128)[:, sl], [("XB", h)], q="pool")
    for g in range(32):
        i = g % 2
        cs = slice(g * 512, (g + 1) * 512)
        h = g // 8
        C.mm(pz[i][0][:], W[:, 0, :], XA[:, cs], True, False, ["W", ("XA", h)], [("pz", i, 0)])
        C.mm(pz[i][0][:], W[:, 2, :], XB[:, cs], False, True, ["W", ("XB", h)], [("pz", i, 0)])
        C.mm(pz[i][1][:], W[:, 2, :], XA[:, cs], True, False, ["W", ("XA", h)], [("pz", i, 1)])
        C.mm(pz[i][1][:], W[:, 3, :], XB[:, cs], False, True, ["W", ("XB", h)], [("pz", i, 1)])
        for k in range(4):
            t2 = g * 4 + k
            ks = slice(k * 128, (k + 1) * 128)
            C.act(ta[i][:, ks], pz[i][0][:, ks], AF.Copy, [("pz", i, 0), "tw"], [("ta", i)], scale=tw[:, 0, t2:t2 + 1])
            C.act(tb_[i][:, ks], pz[i][0][:, ks], AF.Copy, [("pz", i, 0), "tw"], [("tb", i)], scale=tw[:, 1, t2:t2 + 1])
        for k in range(4):
            t2 = g * 4 + k
            ks = slice(k * 128, (k + 1) * 128)
            C.stt("dve", zo[i][0][:, ks], pz[i][1][:, ks], tw[:, 1, t2:t2 + 1], ta[i][:, ks], ALU.mult, ALU.add,
                  [("pz", i, 1), "tw", ("ta", i)], [("zo", i, 0)])
            C.stt("dve", zo[i][1][:, ks], pz[i][1][:, ks], tw[:, 0, t2:t2 + 1], tb_[i][:, ks], ALU.mult, ALU.subtract,
                  [("pz", i, 1), "tw", ("tb", i)], [("zo", i, 1)])
        for x in range(2):
            C.store(ZS[x, :, g * 4:(g + 1) * 4, :], zo[i][x][:].rearrange("p (a c) -> p a c", a=4), [("zo", i, x)], ["ZS"],
                    q="sp" if x == 0 else "pool")
    for x in range(2):
        for h in range(4):
            C.load(ZT[x][:, h * 32:(h + 1) * 32, :], ZS[x, h * 32:(h + 1) * 32, :, :].rearrange("f t c -> t f c"),
                   [("ZT", x, h)], ["ZS"], q="sp" if x == 0 else "pool")
    Y = XA
    Y3 = Y[:].rearrange("p (f2 f1) -> p f2 f1", f1=128)
    for fg in range(32):
        i = fg % 2
        for k in range(4):
            f1 = fg * 4 + k
            ks = slice(k * 128, (k + 1) * 128)
            C.mm(pc[i][:, ks], ZT[0][:, f1, :], W[:, 0, :], k == 0, False, ["W", ("ZT", 0, f1 // 32)], [("pc", i)])
            C.mm(pc[i][:, ks], ZT[1][:, f1, :], W[:, 1, :], False, k == 3, ["W", ("ZT", 1, f1 // 32)], [("pc", i)])
        C.act(Y3[:, :, fg * 4:(fg + 1) * 4], pc[i][:].rearrange("p (a f) -> p f a", a=4), AF.Copy, [("pc", i)],
              [("XA", 0), ("XA", 1), ("XA", 2), ("XA", 3)], scale=1.0 / 2048.0)
    for h in range(4):
        sl = slice(h * 4096, (h + 1) * 4096)
        C.store(yT[:, sl], Y[:, sl], [("XA", h)], ["yT"], q="sp" if h % 2 == 0 else "pool")
    S.emit()
    return C.nc


def fft_tables():
    k = np.arange(128, dtype=np.float64)
    ang = 2 * np.pi * np.outer(k, k) / 128.0
    W = np.stack([np.cos(ang), np.sin(ang), -np.sin(ang), -np.cos(ang)], axis=1).astype(NPBF)
    ang2 = 2 * np.pi * np.outer(k, k) / float(SEQ)
    tw = np.stack([np.cos(ang2), np.sin(ang2)], axis=1).astype(np.float32)
    return np.ascontiguousarray(W), np.ascontiguousarray(tw)


def build_L3(last, NEe=NE, dbg=False):
    C = Ctx()
    S = C.S
    if dbg:
        dbg_mix = C.dout("dbg_mix", [128, 32, 512], BF16)
        dbg_xm = C.dout("dbg_xm", [128, 32, 512])
        dbg_wt = C.dout("dbg_wt", [128, 8192], BF16)
        dbg_wd = C.dout("dbg_wd", [128, 1536], BF16)
    qT = C.din("qT", [2048, TL], BF16)
    kT = C.din("kT", [512, NEXT], BF16)
    vI = C.din("v", [NEXT, 512], BF16)
    kcT = C.din("kcT", [512, LC], BF16)
    vcI = C.din("vc", [LC, 512], BF16)
    convT = C.din("convT", [1024, TL], BF16)
    fourT = C.din("fourT", [1024, TL], BF16)
    xT = C.din("xT", [D, TL])
    if not last:
        qcT = C.din("qcT", [2048, LC], BF16)
        convcT = C.din("convcT", [1024, LC], BF16)
        fourcT = C.din("fourcT", [1024, LC], BF16)
        xcT = C.din("xcT", [D, LC])
        xocT = C.dout("xocT", [D, LC])
    vecd = C.din("vec2", [128, 336])
    maskd = C.din("masks", [128, 4, 512], BF16)
    w_out = C.din("w_out", [D, D])
    wrd = C.din("wr", [128, 32, 32])
    brd = C.din("br", [128, 32])
    w_gu = C.din("w_gu", [NEe, 6, 128, 8192])
    bgud = C.din("bgu", [128, NEe, 12])
    w_dn = C.din("w_dn", [NEe, 128, 24576])
    bdnd = C.din("bdnT", [128, 32, NEe])
    identd = C.din("ident", [128, 128])
    xoT = C.dout("xoT", [D, TL])
    wobf = C.dscr("wobf", [D, D], BF16)
    wgubf = [C.dscr(f"wgubf{i}", [6, 128, 8192], BF16) for i in range(NEe)]
    wdnbf = [C.dscr(f"wdnbf{i}", [128, 24576], BF16) for i in range(NEe)]
    XM = C.dscr("XM", [D, 512])
    GT = C.dscr("GT", [32, 512])

    xt = C.sb("xt", [128, 32, 512])
    mix = C.sb("mix", [128, 32, 512], BF16)
    wt = [C.sb(f"wt{i}", [128, 32, 256], BF16) for i in range(2)]
    wdn = [C.sb(f"wdn{i}", [128, 6, 256], BF16) for i in range(2)]
    actT = C.sb("actT", [128, 6, 512], BF16)
    vec = C.sb("vec", [128, 336])
    gs2L = C.sb("gs2L", [128, 32])
    gs2C = C.sb("gs2C", [128, 32])
    masks = C.sb("masks", [128, 4, 512], BF16)
    wr = C.sb("wr", [128, 32, 32])
    br = C.sb("br", [128, 32])
    bgu = C.sb("bgu", [128, NEe, 12])
    bdn = C.sb("bdnT", [128, 32, NEe])
    ident = C.sb("ident", [128, 128])
    ones = C.sb("ones", [128, 128], BF16)
    zeros = C.sb("zeros", [128, 128])
    sinkrow = C.sb("sinkrow", [128, 16, 128])
    Kc = C.sb("Kc", [128, 4, LC], BF16)
    Vc = C.sb("Vc", [128, 2, 512], BF16)
    Kt = [C.sb(f"Kt{i}", [128, 768], BF16) for i in range(2)]
    Vt = [C.sb(f"Vt{i}", [128, 6, 128], BF16) for i in range(2)]
    Qt0 = C.sb("Qt0", [128, 4, 512], BF16)
    Qt = [Qt0, Qt0]
    Ej = [C.sb(f"E{i}", [128, 512], BF16) for i in range(4)]
    sq = [C.sb(f"sq{i}", [128, 512], BF16) for i in range(2)]
    tmp = [C.sb(f"tmp{i}", [128, 512]) for i in range(4)]
    rstd = C.sb("rstd", [128, 512])
    den = C.sb("den", [128, 512])
    gbs2 = [C.sb(f"gbs{i}", [128, 512]) for i in range(2)]
    gT = C.sb("gT", [32, 512])
    rt = C.sb("rt", [128, 96])
    rt2 = C.sb("rt2", [128, 16])
    bank = [C.ps(f"bank{i}", [128, 512]) for i in range(8)]
    bk = lambda i: ("bank", i)

    C.load(vec[:], vecd, ["vec"])
    C.load(masks[:], maskd, ["masks"])
    C.load(wr[:], wrd, ["wr"])
    C.load(br[:], brd, ["br"])
    C.load(bgu[:], bgud, ["bgu"])
    C.load(bdn[:], bdnd, ["bdn"])
    C.load(ident[:], identd, ["ident"])
    C.load(Kc[:], kcT.rearrange("(h p) t -> p h t", p=128), ["Kc"])
    C.load(Vc[:], vcI.rearrange("(b p) d -> p b d", p=128), ["Vc"])
    S.op("pool", lambda e: e.memset(ones[:], 1.0), writes=["ones"])
    S.op("pool", lambda e: e.memset(zeros[:], 0.0), writes=["zeros"])
    for h in range(16):
        C.act(sinkrow[:, h, :], zeros[:], AF.Exp, ["zeros", "vec"], ["sinkrow"], bias=vec[:, 320 + h:321 + h])
    C.ts("dve", gs2L[:], vec[:, 96:128], 1.0, ALU.add, ["vec"], ["gs2"])
    C.tt("dve", gs2L[:], gs2L[:], vec[:, 32:64], ALU.mult, ["vec", "gs2"], ["gs2"])
    C.ts("dve", gs2C[:], vec[:, 224:256], 1.0, ALU.add, ["vec"], ["gs2"])
    C.tt("dve", gs2C[:], gs2C[:], vec[:, 32:64], ALU.mult, ["vec", "gs2"], ["gs2"])

    xs_ = [xt[:, 0:16, :], xt[:, 16:32, :]]
    it = 0

    def conv_piece(src, dst, width):
        nonlocal it
        s = it % 2
        it += 1
        xf = xs_[s].rearrange("p c t -> p (c t)")[:, 0:width]
        wf = wt[s][:].rearrange("p c t -> p (c t)")[:, 0:width]
        C.load(xf, src, [("cvx", s)])
        C.copy(C.anyeng(), wf, xf, [("cvx", s)], [("wt", s)])
        C.store(dst, wf, [("wt", s)], ["wbf"])
    for kc in range(32):
        conv_piece(w_out[kc * 128:(kc + 1) * 128, :], wobf[kc * 128:(kc + 1) * 128, :], D)
    for e_ in range(NEe):
        for fc in range(6):
            for hh in range(2):
                conv_piece(w_gu[e_, fc, :, hh * 4096:(hh + 1) * 4096], wgubf[e_][fc, :, hh * 4096:(hh + 1) * 4096], 4096)
        for q4 in range(6):
            conv_piece(w_dn[e_, :, q4 * 4096:(q4 + 1) * 4096], wdnbf[e_][:, q4 * 4096:(q4 + 1) * 4096], 4096)
    allxt = [("xt", c) for c in range(32)]
    S.op("dve", lambda e: e.memset(rt2[:, 15:16], 0.0), writes=[("cvx", 0), ("cvx", 1)] + allxt)

    wobf_fm = fm(wobf)
    tiles = [(f"L{i}", 512 * i, 512, "lat") for i in range(4)]
    if not last:
        tiles.append(("C", 0, LC, "ctx"))
    wl = 0
    pa = 0
    ei = 0
    qi = 0
    ti = 0
    wdl = 0
    for name, t0, T, kind in tiles:
        isctx = kind == "ctx"
        xsrc = fm(xcT) if isctx else fm(xT)
        xdst = fm(xocT) if isctx else fm(xoT)
        o_ = 160 if isctx else 0
        gmix = vec[:, 160:192] if isctx else vec[:, 0:32]
        shift2 = vec[:, 192:224] if isctx else vec[:, 64:96]
        gffn = vec[:, 256:288] if isctx else vec[:, 128:160]
        gs2 = gs2C if isctx else gs2L
        for cgi in range(4):
            C.load(xt[:, cgi * 8:(cgi + 1) * 8, 0:T], xsrc[:, cgi * 8:(cgi + 1) * 8, t0:t0 + T],
                   [("xt", c) for c in range(cgi * 8, cgi * 8 + 8)])
        for kvh in range(4):
            ks_ = (kvh + (0 if isctx else 0)) % 2
            if not isctx:
                C.load(Kt[ks_][:], kT[kvh * 128:(kvh + 1) * 128, t0:t0 + 768], [("Kt", ks_)])
                C.load(Vt[ks_][:], vI[t0:t0 + 768, kvh * 128:(kvh + 1) * 128].rearrange("(b p) d -> p b d", p=128), [("Vt", ks_)])
            qsrc = qcT if isctx else qT
            C.load(Qt[ks_][:, :, 0:T], qsrc[kvh * 512:(kvh + 1) * 512, t0:t0 + T].rearrange("(g p) t -> p g t", p=128), [("Qt", 0)])
            for b in range(T // 128):
                blocks = [("ctx", 0), ("ctx", 1)] if isctx else [("loc", 0), ("loc", 1), ("loc", 2), ("ctx", 0), ("ctx", 1)]
                pO = 3 + (qi % 2)
                pD = 5 + (qi % 2)
                qi += 1
                for bi, (bt, jb) in enumerate(blocks):
                    pS = pa % 3
                    pa += 1
                    if bt == "loc":
                        lk = Kt[ks_][:, (b + jb) * 128:(b + jb + 1) * 128]
                        lv = Vt[ks_][:, b + jb, :]
                        rk = [("Kt", ks_)]
                        rv = [("Vt", ks_)]
                    else:
                        lk = Kc[:, kvh, jb * 128:(jb + 1) * 128]
                        lv = Vc[:, jb, kvh * 128:(kvh + 1) * 128]
                        rk = ["Kc"]
                        rv = ["Vc"]
                    C.mm(bank[pS][:].rearrange("p (g q) -> p g q", g=4), lk, Qt[ks_][:, :, b * 128:(b + 1) * 128], True, True,
                         rk + [("Qt", 0)], [bk(pS)])
                    E = Ej[ei % 4]
                    ek = ("E", ei % 4)
                    ei += 1
                    C.act(E[:], bank[pS][:], AF.Exp, [bk(pS)], [ek], scale=float(128 ** -0.5))
                    if bt == "loc" and jb != 1:
                        if jb == 0:
                            mi = 0 if (name == "L0" and b == 0) else 1
                        else:
                            mi = 3 if (name == "L3" and b == 3) else 2
                        C.tt("pool", E[:], E[:], masks[:, mi, :], ALU.mult, [ek, "masks"], [ek])
                    C.mm(bank[pO][:], lv, E[:], bi == 0, bi == len(blocks) - 1, rv + [ek], [bk(pO)])
                    C.mm(bank[pD][:], ones[:], E[:], bi == 0, bi == len(blocks) - 1, ["ones", ek], [bk(pD)])
                C.tt("dve", den[:], bank[pD][:], sinkrow[:, kvh * 4:(kvh + 1) * 4, :].rearrange("p g q -> p (g q)"), ALU.add,
                     [bk(pD), "sinkrow"], ["den"])
                S.op("dve", lambda e: e.reciprocal(out=den[:], in_=den[:]), reads=["den"], writes=["den"])
                C.tt("dve", mix[:, kvh * 4:(kvh + 1) * 4, b * 128:(b + 1) * 128], bank[pO][:].rearrange("p (g q) -> p g q", g=4),
                     den[:].rearrange("p (g q) -> p g q", g=4), ALU.mult, [bk(pO), "den"], ["mix"])
        csrc, fsrc = (convcT, fourcT) if isctx else (convT, fourT)
        C.load(mix[:, 16:24, 0:T], fm(csrc)[:, :, t0:t0 + T], ["mix"])
        C.load(mix[:, 24:32, 0:T], fm(fsrc)[:, :, t0:t0 + T], ["mix"])
        if dbg and name == "L0":
            C.store(dbg_mix, mix[:], ["mix"], ["dbgmix"])
        for g in range(16):
            ws = wl % 2
            wl += 1
            C.load(wt[ws][:], wobf_fm[:, :, g * 256:(g + 1) * 256], [("wt", ws)], ["wbf"])
            for j in range(2):
                n = 2 * g + j
                p = pa % 3
                pa += 1
                for kc in range(32):
                    C.mm(bank[p][:, 0:T], wt[ws][:, kc, j * 128:(j + 1) * 128], mix[:, kc, 0:T], kc == 0, kc == 31,
                         ["mix", ("wt", ws)], [bk(p)])
                C.stt("dve", xt[:, n, 0:T], bank[p][:, 0:T], gmix[:, n:n + 1], xt[:, n, 0:T], ALU.mult, ALU.add,
                      [bk(p), "vec", ("xt", n)], [("xt", n)])
        if dbg and name == "L0":
            C.store(dbg_xm, xt[:], allxt, ["dbgxm"])
        for c in range(32):
            q = c % 2
            C.act(sq[q][:, 0:T], xt[:, c, 0:T], AF.Square, [("xt", c)], [("sq", q)])
            C.mm(bank[7][:, 0:T], ones[:], sq[q][:, 0:T], c == 0, c == 31, [("sq", q), "ones"], [bk(7)])
        C.ts("dve", rstd[:, 0:T], bank[7][:, 0:T], 1.0 / D, ALU.mult, [bk(7)], ["rstd"], s2=EPS, op1=ALU.add)
        C.act(rstd[:, 0:T], rstd[:, 0:T], AF.Sqrt, ["rstd"], ["rstd"])
        S.op("dve", lambda e, T=T: e.reciprocal(out=rstd[:, 0:T], in_=rstd[:, 0:T]), reads=["rstd"], writes=["rstd"])
        nb = T // 128
        for c in range(32):
            q = ti % 2
            q2 = 2 + ti % 2
            ti += 1
            C.tt("dve", tmp[q][:, 0:T], xt[:, c, 0:T], rstd[:, 0:T], ALU.mult, [("xt", c), "rstd"], [("tmp", q)])
            C.act(tmp[q2][:, 0:T], tmp[q][:, 0:T], AF.Identity, [("tmp", q), "vec", "gs2"], [("tmp", q2)],
                  bias=shift2[:, c:c + 1], scale=gs2[:, c:c + 1])
            C.copy("pool", mix[:, c, 0:T], tmp[q2][:, 0:T], [("tmp", q2)], ["mix"])
            for tb in range(nb):
                C.mm(bank[3 + tb][:, 0:32], tmp[q2][:, tb * 128:(tb + 1) * 128], wr[:, c, :],
                     c == 0, c == 31, [("tmp", q2), "wr"], [bk(3 + tb)])
        for cgi in range(4):
            C.store(fm(XM)[:, cgi * 8:(cgi + 1) * 8, 0:T], xt[:, cgi * 8:(cgi + 1) * 8, 0:T],
                    [("xt", c) for c in range(cgi * 8, cgi * 8 + 8)], [("XM", cgi)], q="sp")
        for tb in range(nb):
            C.tt("dve", rt[:, 0:32], bank[3 + tb][:, 0:32], br[:], ALU.add, [bk(3 + tb), "br"], ["rt"])
            S.op("dve", lambda e: e.max(out=rt2[:, 0:8], in_=rt[:, 0:32]), reads=["rt"], writes=["rt2"])
            C.ts("dve", rt[:, 64:96], rt[:, 0:32], rt2[:, 3:4], ALU.is_ge, ["rt", "rt2"], ["rtm"])
            C.ts("dve", rt2[:, 8:9], rt2[:, 0:1], -1.0, ALU.mult, ["rt2"], ["rt2n"])
            C.act(rt[:, 32:64], rt[:, 0:32], AF.Exp, ["rt", "rt2n"], ["rte"], bias=rt2[:, 8:9])
            C.tt("dve", rt[:, 32:64], rt[:, 32:64], rt[:, 64:96], ALU.mult, ["rte", "rtm"], ["rte"])
            S.op("dve", lambda e: e.reduce_sum(out=rt2[:, 9:10], in_=rt[:, 32:64], axis=mybir.AxisListType.X),
                 reads=["rte"], writes=["rt2s"])
            S.op("dve", lambda e: e.reciprocal(out=rt2[:, 10:11], in_=rt2[:, 9:10]), reads=["rt2s"], writes=["rt2r"])
            C.ts("dve", rt[:, 32:64], rt[:, 32:64], rt2[:, 10:11], ALU.mult, ["rte", "rt2r"], ["rte"])
            gb_ = (0, 1, 2, 7)[tb]
            C.mm(bank[gb_][0:32, 0:128], rt[:, 32:64], ident[:], True, True, ["rte", "ident"], [bk(gb_)])
            C.copy("act", gT[:, tb * 128:(tb + 1) * 128], bank[gb_][0:32, 0:128], [bk(gb_)], ["gT"])
        C.store(GT[:, 0:T], gT[:, 0:T], ["gT"], ["GT"], q="sp")
        S.op("pool", lambda e, T=T: e.memset(xt[:, :, 0:T], 0.0), reads=[("XM", i) for i in range(4)], writes=allxt)
        for e_ in range(NEe):
            gbs = gbs2[e_ % 2]
            gk = ("gbs", e_ % 2)
            for hh in range(T // 256):
                C.load(gbs[:, hh * 256:(hh + 1) * 256], GT[e_:e_ + 1, hh * 256:(hh + 1) * 256].partition_broadcast(128), [gk], ["GT"])
            for fc in range(6):
                ws = wl % 2
                wl += 1
                C.load(wt[ws][:].rearrange("p c t -> p (c t)"), wgubf[e_][fc], [("wt", ws)], ["wbf"])
                wv = wt[ws][:].rearrange("p c t -> p (c t)").rearrange("p (g k j) -> p g k j", g=2, k=32)
                if dbg and name == "L0" and e_ == 1 and fc == 2:
                    C.store(dbg_wt, wt[ws][:].rearrange("p c t -> p (c t)"), [("wt", ws)], ["dbgwt"])
                pgt = pa % 3
                pa += 1
                put = pa % 3
                pa += 1
                for kc in range(32):
                    C.mm(bank[pgt][:, 0:T], wv[:, 0, kc, :], mix[:, kc, 0:T], kc == 0, kc == 31, ["mix", ("wt", ws)], [bk(pgt)])
                for kc in range(32):
                    C.mm(bank[put][:, 0:T], wv[:, 1, kc, :], mix[:, kc, 0:T], kc == 0, kc == 31, ["mix", ("wt", ws)], [bk(put)])
                g1, sg, u1 = tmp[0], tmp[1], tmp[2]
                C.ts("dve", g1[:, 0:T], bank[pgt][:, 0:T], bgu[:, e_, fc:fc + 1], ALU.add, [bk(pgt), "bgu"], [("tmp", 0)], s2=7.0, op1=ALU.min)
                C.act(sg[:, 0:T], g1[:, 0:T], AF.Sigmoid, [("tmp", 0)], [("tmp", 1)], scale=1.702)
                C.ts("dve", u1[:, 0:T], bank[put][:, 0:T], bgu[:, e_, 6 + fc:7 + fc], ALU.add, [bk(put), "bgu"], [("tmp", 2)], s2=7.0, op1=ALU.min)
                C.ts("pool", u1[:, 0:T], u1[:, 0:T], -7.0, ALU.max, [("tmp", 2)], [("tmp", 2)], s2=1.0, op1=ALU.add)
                C.tt("pool", sg[:, 0:T], sg[:, 0:T], g1[:, 0:T], ALU.mult, [("tmp", 0), ("tmp", 1)], [("tmp", 1)])
                C.tt("dve", sg[:, 0:T], sg[:, 0:T], u1[:, 0:T], ALU.mult, [("tmp", 1), ("tmp", 2)], [("tmp", 1)])
                C.tt("pool", actT[:, fc, 0:T], sg[:, 0:T], gbs[:, 0:T], ALU.mult, [("tmp", 1), gk], [("actT", fc)])
            for ng in range(16):
                ds_ = wdl % 2
                wdl += 1
                C.load(wdn[ds_][:].rearrange("p f j -> p (f j)"), wdnbf[e_][:, ng * 1536:(ng + 1) * 1536], [("wdn", ds_)], ["wbf"])
                if dbg and name == "L0" and e_ == 1 and ng == 3:
                    C.store(dbg_wd, wdn[ds_][:].rearrange("p f j -> p (f j)"), [("wdn", ds_)], ["dbgwd"])
                for j in range(2):
                    n = ng * 2 + j
                    p = pa % 3
                    pa += 1
                    for fc in range(6):
                        C.mm(bank[p][:, 0:T], wdn[ds_][:, fc, j * 128:(j + 1) * 128], actT[:, fc, 0:T], fc == 0, fc == 5,
                             [("wdn", ds_), ("actT", fc)], [bk(p)])
                    C.tt("dve", xt[:, n, 0:T], xt[:, n, 0:T], bank[p][:, 0:T], ALU.add, [bk(p), ("xt", n)], [("xt", n)])
                    C.stt("dve", xt[:, n, 0:T], gbs[:, 0:T], bdn[:, n, e_:e_ + 1], xt[:, n, 0:T], ALU.mult, ALU.add,
                          [gk, "bdn", ("xt", n)], [("xt", n)])
        for n in range(32):
            q = ti % 4
            ti += 1
            C.load(tmp[q][:, 0:T], fm(XM)[:, n, 0:T], [("tmp", q)], [("XM", n // 8)])
            C.stt("dve", xt[:, n, 0:T], xt[:, n, 0:T], gffn[:, n:n + 1], tmp[q][:, 0:T], ALU.mult, ALU.add,
                  [("xt", n), "vec", ("tmp", q)], [("xt", n)])
        if not last:
            for cgi in range(4):
                C.store(xdst[:, cgi * 8:(cgi + 1) * 8, t0:t0 + T], xt[:, cgi * 8:(cgi + 1) * 8, 0:T],
                        [("xt", c) for c in range(cgi * 8, cgi * 8 + 8)], ["xout"], q="sp")
        else:
            for c in range(32):
                q = c % 2
                C.act(sq[q][:, 0:T], xt[:, c, 0:T], AF.Square, [("xt", c)], [("sq", q)])
                C.mm(bank[7][:, 0:T], ones[:], sq[q][:, 0:T], c == 0, c == 31, [("sq", q), "ones"], [bk(7)])
            C.ts("dve", rstd[:, 0:T], bank[7][:, 0:T], 1.0 / D, ALU.mult, [bk(7)], ["rstd"], s2=EPS, op1=ALU.add)
            C.act(rstd[:, 0:T], rstd[:, 0:T], AF.Sqrt, ["rstd"], ["rstd"])
            S.op("dve", lambda e, T=T: e.reciprocal(out=rstd[:, 0:T], in_=rstd[:, 0:T]), reads=["rstd"], writes=["rstd"])
            for c in range(32):
                q = ti % 4
                ti += 1
                C.tt("dve", tmp[q][:, 0:T], xt[:, c, 0:T], rstd[:, 0:T], ALU.mult, [("xt", c), "rstd"], [("tmp", q)])
                C.act(xt[:, c, 0:T], tmp[q][:, 0:T], AF.Copy, [("tmp", q), "vec"], [("xt", c)], scale=vec[:, 288 + c:289 + c])
            for cgi in range(4):
                C.store(xdst[:, cgi * 8:(cgi + 1) * 8, t0:t0 + T], xt[:, cgi * 8:(cgi + 1) * 8, 0:T],
                        [("xt", c) for c in range(cgi * 8, cgi * 8 + 8)], ["xout"], q="sp")
    S.emit()
    return C.nc


def make_vec2(modL, modC, g_ffn, g_final, sink):
    v = np.zeros((128, 336), np.float32)
    v[:, 0:32] = fmvec(modL[2])
    v[:, 32:64] = fmvec(g_ffn)
    v[:, 64:96] = fmvec(modL[3])
    v[:, 96:128] = fmvec(modL[4])
    v[:, 128:160] = fmvec(modL[5])
    v[:, 160:192] = fmvec(modC[2])
    v[:, 192:224] = fmvec(modC[3])
    v[:, 224:256] = fmvec(modC[4])
    v[:, 256:288] = fmvec(modC[5])
    v[:, 288:320] = fmvec(g_final)
    v[:, 320:336] = np.asarray(sink, np.float32)[None, :]
    return v


def make_masks(j):
    k = np.arange(128)[:, None]
    q = np.arange(128)[None, :]
    mp = np.tile((k >= q).astype(np.float32), (1, 4))
    mn = np.tile((k <= q).astype(np.float32), (1, 4))
    z = np.zeros_like(mp)
    m = np.stack([z if j == 0 else mp, mp, mn, z if j == NCORES - 1 else mn], axis=1)
    return np.ascontiguousarray(m).astype(NPBF)


def expert_layout(w_gu_l, w_dn_l):
    ne = w_gu_l.shape[0]
    a = w_gu_l.reshape(ne, 32, 128, 2, 6, 128).transpose(0, 4, 2, 3, 1, 5)
    a = np.ascontiguousarray(a).reshape(ne, 6, 128, 8192)
    b = w_dn_l.reshape(ne, 6, 128, 16, 256).transpose(0, 2, 3, 1, 4)
    b = np.ascontiguousarray(b).reshape(ne, 128, 24576)
    return a, b


def moe_small_inputs(w_router, b_router, b_gu, b_dn, ne=NE):
    wr = np.ascontiguousarray(np.asarray(w_router, np.float32).reshape(32, 128, 32).transpose(1, 0, 2))
    br = np.ascontiguousarray(np.broadcast_to(np.asarray(b_router, np.float32)[None, :], (128, 32)))
    bgu = np.ascontiguousarray(np.asarray(b_gu, np.float32)[:ne].reshape(ne, 12, 128).transpose(2, 0, 1))
    bdnT = np.ascontiguousarray(np.asarray(b_dn, np.float32)[:ne].reshape(ne, 32, 128).transpose(2, 1, 0))
    return wr, br, bgu, bdnT


def _run(nc, in_maps):
    return run_bass_kernel_spmd(nc, in_maps, core_ids=list(range(NCORES))).results


def ext_from_fm(xfm_list):
    full = np.concatenate(xfm_list, axis=1)
    pad = np.zeros((full.shape[0], SEQ + 256), full.dtype)
    pad[:, 128:128 + SEQ] = full
    return [np.ascontiguousarray(pad[:, j * TL:j * TL + NEXT]) for j in range(NCORES)]


def kernel(x, c, ctx, c_ctx, w_ada, b_ada, g_mix, w_in, conv_w, attn_sink, w_out, g_ffn,
           w_router, b_router, w_gu, b_gu, w_dn, b_dn, g_final):
    f = lambda a: np.asarray(a, np.float32)
    x, c, ctx, c_ctx = f(x), f(c), f(ctx), f(c_ctx)
    w_ada, b_ada, g_mix, w_in, conv_w, attn_sink = f(w_ada), f(b_ada), f(g_mix), f(w_in), f(conv_w), f(attn_sink)
    w_out, g_ffn, w_router, b_router = f(w_out), f(g_ffn), f(w_router), f(b_router)
    w_gu, b_gu, w_dn, b_dn, g_final = f(w_gu), f(b_gu), f(w_dn), f(b_dn), f(g_final)
    R = range(NCORES)
    cond = np.ascontiguousarray(np.stack([c[0], c_ctx], 0).reshape(2, 32, 128).transpose(2, 1, 0))
    res = _run(build_L0(), [{"cond": cond, "wada": np.ascontiguousarray(w_ada[:, :, j * NA:(j + 1) * NA]),
                             "bada": np.ascontiguousarray(np.broadcast_to(b_ada[:, None, j * NA:(j + 1) * NA], (2, 2, NA)))}
                            for j in R])
    mod = np.concatenate([res[j]["mod"] for j in R], axis=2).reshape(2, 2, 6, D)
    cos, sin, perm = rope_tables()
    cosp = np.zeros((128, SEQ + 256), np.float32)
    cosp[:, 128:128 + SEQ] = cos
    sinp = np.zeros((128, SEQ + 256), np.float32)
    sinp[:, 128:128 + SEQ] = sin
    dft = dft256_table()
    Wf, twf = fft_tables()
    ident = np.eye(128, dtype=np.float32)
    nc2 = build_L2()
    xfm = [np.ascontiguousarray(x[0, j * TL:(j + 1) * TL].T) for j in R]
    xcfm = np.ascontiguousarray(ctx[0].T)
    for l in range(2):
        last = l == 1
        modL, modC = mod[l, 0], mod[l, 1]
        xext = ext_from_fm(xfm)
        r1 = _run(build_L1(last), [{
            "xT": xext[j], "xcT": xcfm,
            "vec": make_vec(j, g_mix[l], modL[0], modL[1], modC[0], modC[1], conv_w[l]),
            "w_in": w_in[l], "ropec": np.ascontiguousarray(cosp[:, j * TL:j * TL + NEXT]),
            "ropes": np.ascontiguousarray(sinp[:, j * TL:j * TL + NEXT]), "perm": perm, "dft": dft} for j in R])
        del xext
        A = np.concatenate([r1[j]["AB"][0] for j in R], axis=0)
        B = np.concatenate([r1[j]["AB"][1] for j in R], axis=0)
        r2 = _run(nc2, [{"Ain": np.ascontiguousarray(A[:, j * 128:(j + 1) * 128]),
                         "Bin": np.ascontiguousarray(B[:, j * 128:(j + 1) * 128]), "W": Wf, "tw": twf} for j in R])
        four = np.concatenate([r2[j]["yT"] for j in R], axis=0)
        del A, B, r2
        wr, br, bgu, bdnT = moe_small_inputs(w_router[l], b_router[l], b_gu[l], b_dn[l])
        vec2 = make_vec2(modL, modC, g_ffn[l], g_final, attn_sink[l])
        wgu_l, wdn_l = expert_layout(w_gu[l], w_dn[l])
        maps = []
        for j in R:
            m = {"qT": r1[j]["qT"], "kT": r1[j]["kT"], "v": r1[j]["v"], "kcT": r1[j]["kcT"], "vc": r1[j]["vc"],
                 "convT": r1[j]["convT"], "fourT": np.ascontiguousarray(four[:, j * TL:(j + 1) * TL]), "xT": xfm[j],
                 "vec2": vec2, "masks": make_masks(j), "w_out": w_out[l], "wr": wr, "br": br, "w_gu": wgu_l,
                 "bgu": bgu, "w_dn": wdn_l, "bdnT": bdnT, "ident": ident}
            if not last:
                m.update({"qcT": r1[j]["qcT"], "convcT": r1[j]["convcT"], "fourcT": r1[j]["fourcT"], "xcT": xcfm})
            maps.append(m)
        r3 = _run(build_L3(last), maps)
        del maps, r1, four
        xfm = [r3[j]["xoT"] for j in R]
        if not last:
            xcfm = r3[0]["xocT"]
        del r3
    out = np.concatenate([xfm[j].T for j in R], axis=0)[None]
    return np.ascontiguousarray(out.astype(np.float32))
```

```python
import numpy as np
import ml_dtypes
import concourse.bass as bass
import concourse.mybir as mybir
from concourse.bass_utils import run_bass_kernel_spmd

F32 = mybir.dt.float32
BF16 = mybir.dt.bfloat16
AF = mybir.ActivationFunctionType
ALU = mybir.AluOpType
NPBF = ml_dtypes.bfloat16

NCORES = 8
D = 4096
SEQ = 16384
TL = SEQ // NCORES
NEXT = TL + 256
LC = 256
INW = 7168
NE = 32
DE = 768
EPS = 1e-5


class Sched:
    ENGS = ("pe", "act", "dve", "pool", "sp")
    NDMASEM = 8

    def __init__(self, nc):
        self.nc = nc
        self.cnt = {e: 0 for e in self.ENGS}
        self.known = {e: {} for e in self.ENGS}
        self.prog = {e: [] for e in self.ENGS}
        self.snap = {}
        self.dval = {}
        self.drr = {}
        self.lastw = {}
        self.readers = {}

    def _learn(self, e, ev):
        k = self.known[e]
        if ev[0] == "c":
            sn = self.snap.get((ev[1], ev[2]))
            key, val = ("c", ev[1]), ev[2]
        else:
            sn = ev[3]
            key, val = ("d", ev[1]), ev[2]
        if k.get(key, 0) < val:
            k[key] = val
        if sn:
            for kk, vv in sn.items():
                if k.get(kk, 0) < vv:
                    k[kk] = vv

    def _deps(self, reads, writes):
        evs = []
        for r in reads:
            w = self.lastw.get(r)
            if w is not None:
                evs.append(w)
        for r in writes:
            w = self.lastw.get(r)
            if w is not None:
                evs.append(w)
            evs.extend(self.readers.get(r, ()))
        return evs

    def _emit_waits(self, e, evs):
        waits = {}
        for ev in evs:
            if ev[0] == "c":
                key, val = ("c", ev[1]), ev[2]
            else:
                key, val = ("d", ev[1]), ev[2]
            if self.known[e].get(key, 0) >= val:
                continue
            if waits.get(key, (0, None))[0] < val:
                waits[key] = (val, ev)
        for key, (val, ev) in waits.items():
            if self.known[e].get(key, 0) >= val:
                continue
            self.prog[e].append(("w", key, val))
            self._learn(e, ev)

    def _record(self, ev, reads, writes):
        for r in writes:
            self.lastw[r] = ev
            self.readers[r] = []
        for r in reads:
            if r not in writes:
                self.readers.setdefault(r, []).append(ev)

    def op(self, e, fn, reads=(), writes=()):
        self._emit_waits(e, self._deps(reads, writes))
        self.cnt[e] += 1
        idx = self.cnt[e]
        self.prog[e].append(("o", fn, idx))
        if e == "pe":
            self.known[e][("c", e)] = idx
        self.snap[(e, idx)] = dict(self.known[e])
        ev = ("c", e, idx)
        self._record(ev, reads, writes)
        return ev

    def dma(self, q, fn, reads=(), writes=(), pre="", nsem=None):
        nsem = nsem or self.NDMASEM
        slot = self.drr.get(pre + q, 0) % nsem
        self.drr[pre + q] = self.drr.get(pre + q, 0) + 1
        skey = f"{pre}{q}{slot}"
        prev = self.dval.get(skey, 0)
        evs = self._deps(reads, writes)
        if prev:
            evs.append(("d", skey, prev, None))
        self._emit_waits(q, evs)
        val = prev + 16
        self.dval[skey] = val
        self.prog[q].append(("d", fn, skey))
        ev = ("d", skey, val, dict(self.known[q]))
        self._record(ev, reads, writes)
        return ev

    def finish(self):
        evs = [("c", f, self.cnt[f]) for f in ("pe", "act", "dve", "pool") if self.cnt[f]]
        for skey, v in self.dval.items():
            evs.append(("d", skey, v, None))
        self._emit_waits("sp", evs)

    def emit(self):
        import contextlib
        nc = self.nc
        self.finish()
        with contextlib.ExitStack() as st:
            semh = {}
            for e in ("pe", "act", "dve", "pool"):
                semh[("c", e)] = st.enter_context(nc.semaphore(f"c_{e}"))
            for skey in self.dval:
                semh[("d", skey)] = st.enter_context(nc.semaphore(f"d_{skey}"))
            st.enter_context(nc.allow_non_contiguous_dma(reason="small strided pieces are intentional"))
            block = st.enter_context(nc.Block())

            def run(e):
                def body(engine):
                    for it in self.prog[e]:
                        if it[0] == "w":
                            engine.wait_ge(semh[it[1]], it[2])
                        elif it[0] == "o":
                            it[1](engine).then_inc(semh[("c", e)], 1)
                        else:
                            it[1](engine).then_inc(semh[("d", it[2])], 16)
                return body
            block.tensor(run("pe"))
            block.scalar(run("act"))
            block.vector(run("dve"))
            block.gpsimd(run("pool"))
            block.sync(run("sp"))


class Ctx:
    def __init__(self):
        self.nc = bass.Bass("TRN2", target_bir_lowering=False)
        self.S = Sched(self.nc)
        self.rr = 0

    def din(self, name, shape, dt=F32):
        return self.nc.dram_tensor(name, list(shape), dt, kind="ExternalInput").ap()

    def dout(self, name, shape, dt=F32):
        return self.nc.dram_tensor(name, list(shape), dt, kind="ExternalOutput").ap()

    def dscr(self, name, shape, dt=F32):
        return self.nc.dram_tensor(name, list(shape), dt).ap()

    def sb(self, name, shape, dt=F32):
        return self.nc.alloc_sbuf_tensor("sb_" + name, list(shape), dt)

    def ps(self, name, shape, dt=F32):
        return self.nc.alloc_psum_tensor("ps_" + name, list(shape), dt)

    def load(self, out, in_, w, r=(), q="sp"):
        return self.S.dma(q, lambda e: e.dma_start(out=out, in_=in_), reads=list(r), writes=list(w))

    def store(self, out, in_, r, w=(), q="pool"):
        return self.S.dma(q, lambda e: e.dma_start(out=out, in_=in_), reads=list(r), writes=list(w))

    def castdma(self, out, in_, w, r=()):
        return self.S.dma("pool", lambda e: e.dma_start(out=out, in_=in_), reads=list(r), writes=list(w), pre="cv", nsem=40)

    def mm(self, out, lhsT, rhs, start, stop, r, w):
        return self.S.op("pe", lambda e: e.matmul(out, lhsT=lhsT, rhs=rhs, start=start, stop=stop),
                         reads=list(r), writes=list(w))

    def act(self, out, in_, func, r, w, bias=None, scale=None):
        kw = {}
        if bias is not None:
            kw["bias"] = bias
        if scale is not None:
            kw["scale"] = scale
        return self.S.op("act", lambda e: e.activation(out=out, in_=in_, func=func, **kw),
                         reads=list(r), writes=list(w))

    def tt(self, eng, out, in0, in1, op, r, w):
        return self.S.op(eng, lambda e: e.tensor_tensor(out=out, in0=in0, in1=in1, op=op),
                         reads=list(r), writes=list(w))

    def ts(self, eng, out, in0, s1, op0, r, w, s2=None, op1=None):
        if op1 is None:
            return self.S.op(eng, lambda e: e.tensor_scalar(out=out, in0=in0, scalar1=s1, scalar2=None, op0=op0),
                             reads=list(r), writes=list(w))
        return self.S.op(eng, lambda e: e.tensor_scalar(out=out, in0=in0, scalar1=s1, scalar2=s2, op0=op0, op1=op1),
                         reads=list(r), writes=list(w))

    def stt(self, eng, out, in0, scalar, in1, op0, op1, r, w):
        return self.S.op(eng, lambda e: e.scalar_tensor_tensor(out=out, in0=in0, scalar=scalar, in1=in1, op0=op0, op1=op1),
                         reads=list(r), writes=list(w))

    def copy(self, eng, out, in_, r, w):
        if eng == "act":
            return self.act(out, in_, AF.Copy, r, w)
        return self.S.op(eng, lambda e: e.tensor_copy(out=out, in_=in_), reads=list(r), writes=list(w))

    def anyeng(self, choices=("dve", "pool", "act")):
        self.rr += 1
        return choices[self.rr % len(choices)]


def fm(ap):
    return ap.rearrange("(c p) t -> p c t", p=128)


NA = 24576 // NCORES


def build_L0():
    C = Ctx()
    S = C.S
    cond = C.din("cond", [128, 32, 2])
    wada = C.din("wada", [2, D, NA])
    bada = C.din("bada", [2, 2, NA])
    mod = C.dout("mod", [2, 2, NA])
    sc = C.sb("sc", [128, 32, 2])
    bt = C.sb("bt", [2, 2, NA])
    res = C.sb("res", [2, 2, NA])
    wt = [C.sb(f"wt{i}", [128, NA]) for i in range(3)]
    pss = [C.ps(f"ps{i}", [2, 512]) for i in range(6)]
    C.load(sc[:], cond, ["sc"])
    C.load(bt[:], bada.rearrange("l i n -> i l n"), ["bt"])
    C.act(sc[:], sc[:], AF.Silu, ["sc"], ["sc"])
    for l in range(2):
        for kc in range(32):
            s = (l * 32 + kc) % 3
            C.load(wt[s][:], wada[l, kc * 128:(kc + 1) * 128, :], [("wt", s)])
            for g in range(6):
                C.mm(pss[g][:], sc[:, kc, :], wt[s][:, g * 512:(g + 1) * 512], kc == 0, kc == 31,
                     ["sc", ("wt", s)], [("ps", g)])
        for g in range(6):
            C.tt("dve", res[:, l, g * 512:(g + 1) * 512], pss[g][:], bt[:, l, g * 512:(g + 1) * 512], ALU.add,
                 [("ps", g), "bt"], ["res"])
    C.store(mod.rearrange("l i n -> i l n"), res[:], ["res"], ["mod"], q="sp")
    S.emit()
    return C.nc


def convert_weights(C, src, dst, rows, cols, xs, wt, piece=3584):
    it = 0
    for kc in range(rows // 128):
        for c0 in range(0, cols, piece):
            w = min(piece, cols - c0)
            s = it % 2
            it += 1
            xf = xs[s][:].rearrange("p c t -> p (c t)")[:, 0:w]
            wf = wt[s][:].rearrange("p c t -> p (c t)")[:, 0:w]
            C.load(xf, src[kc * 128:(kc + 1) * 128, c0:c0 + w], [("xs", s)])
            C.copy(C.anyeng(), wf, xf, [("xs", s)], [("wt", s)])
            C.store(dst[kc * 128:(kc + 1) * 128, c0:c0 + w], wf, [("wt", s)], ["wbf"])


def rms_modulate(C, src_fm, t0, T, xs, hT, sq, ssq, rstd, tmp, ones, gs, shift, h32=None):
    for cgi in range(4):
        s = cgi % 2
        C.load(xs[s][:, :, 0:T], src_fm[:, cgi * 8:(cgi + 1) * 8, t0:t0 + T], [("xs", s)])
        for c in range(8):
            q = c % 2
            C.act(sq[q][:, 0:T], xs[s][:, c, 0:T], AF.Square, [("xs", s)], [("sq", q)])
            C.mm(ssq[:, 0:T], ones[:], sq[q][:, 0:T], cgi == 0 and c == 0, cgi == 3 and c == 7,
                 [("sq", q), "ones"], ["ssq"])
    C.ts("dve", rstd[:, 0:T], ssq[:, 0:T], 1.0 / D, ALU.mult, ["ssq"], ["rstd"], s2=EPS, op1=ALU.add)
    C.act(rstd[:, 0:T], rstd[:, 0:T], AF.Sqrt, ["rstd"], ["rstd"])
    C.S.op("dve", lambda e, T=T: e.reciprocal(out=rstd[:, 0:T], in_=rstd[:, 0:T]), reads=["rstd"], writes=["rstd"])
    for cgi in range(4):
        s = cgi % 2
        C.load(xs[s][:, :, 0:T], src_fm[:, cgi * 8:(cgi + 1) * 8, t0:t0 + T], [("xs", s)])
        for c in range(8):
            cc = cgi * 8 + c
            q = c % 2
            C.tt("dve", tmp[q][:, 0:T], xs[s][:, c, 0:T], rstd[:, 0:T], ALU.mult, [("xs", s), "rstd"], [("tmp", q)])
            C.act(hT[:, cc, 0:T], tmp[q][:, 0:T], AF.Identity, [("tmp", q), "vec"], ["hT"],
                  bias=shift[:, cc:cc + 1], scale=gs[:, cc:cc + 1])


def build_L1(last, dbg=False):
    C = Ctx()
    S = C.S
    if dbg:
        dbg_h = C.dout("dbg_h", [128, 32, 256], BF16)
        dbg_r = C.dout("dbg_r", [128, 256])
        dbg_w = C.dout("dbg_w", [128, 32, 512], BF16)
    xT = C.din("xT", [D, NEXT])
    xcT = C.din("xcT", [D, LC])
    vecd = C.din("vec", [128, 192])
    w_in = C.din("w_in", [D, INW])
    ropec = C.din("ropec", [128, NEXT])
    ropes = C.din("ropes", [128, NEXT])
    permd = C.din("perm", [128, 128])
    dftd = C.din("dft", [128, 2, 3, 256], BF16)
    qT = C.dout("qT", [2048, TL], BF16)
    kT = C.dout("kT", [512, NEXT], BF16)
    vO = C.dout("v", [NEXT, 512], BF16)
    kcT = C.dout("kcT", [512, LC], BF16)
    vcO = C.dout("vc", [LC, 512], BF16)
    convT = C.dout("convT", [1024, TL], BF16)
    AB = C.dout("AB", [2, TL, 1024], BF16)
    if not last:
        qcT = C.dout("qcT", [2048, LC], BF16)
        convcT = C.dout("convcT", [1024, LC], BF16)
        fourcT = C.dout("fourcT", [1024, LC], BF16)
    wbf = C.dscr("wbf", [D, INW], BF16)
    Z = C.dscr("Z", [1024, NEXT])
    BG = C.dscr("BG", [1024, NEXT], BF16)
    Zc = C.dscr("Zc", [1024, LC + 2])
    BGc = C.dscr("BGc", [1024, LC], BF16)

    xs = [C.sb(f"xs{i}", [128, 8, 512]) for i in range(2)]
    wt = [C.sb(f"wt{i}", [128, 32, 512], BF16) for i in range(2)]
    hT = C.sb("hT", [128, 32, 512], BF16)
    cgs = C.sb("cgs", [128, 8, 512])
    fuT = C.sb("fuT", [128, 8, 512], BF16)
    vec = C.sb("vecs", [128, 192])
    gsL = C.sb("gsL", [128, 32])
    gsC = C.sb("gsC", [128, 32])
    ones = C.sb("ones", [128, 128], BF16)
    perm = C.sb("perm", [128, 128])
    dft = C.sb("dfts", [128, 2, 3, 256], BF16)
    sq = [C.sb(f"sq{i}", [128, 512], BF16) for i in range(2)]
    tmp = [C.sb(f"tmp{i}", [128, 512]) for i in range(2)]
    rstd = C.sb("rstd", [128, 512])
    rc = C.sb("rc", [128, 512])
    rs = C.sb("rs", [128, 512])
    xq = [C.sb(f"xq{i}", [128, 512]) for i in range(2)]
    st16 = [C.sb(f"st16_{i}", [128, 512], BF16) for i in range(3)]
    st32 = [C.sb(f"st32_{i}", [128, 514]) for i in range(2)]
    zero = C.sb("zero", [128, 2])
    abc = C.sb("abc", [128, 2, 2, 1024], BF16)
    ssq = C.ps("ssq", [128, 512])
    pacc = [C.ps(f"pacc{i}", [128, 512]) for i in range(3)]
    pperm = [C.ps(f"pperm{i}", [128, 512]) for i in range(2)]

    C.load(vec[:], vecd, ["vec"])
    C.load(perm[:], permd, ["perm"])
    C.load(dft[:], dftd, ["dft"])
    S.op("pool", lambda e: e.memset(ones[:], 1.0), writes=["ones"])
    S.op("pool", lambda e: e.memset(zero[:], 0.0), writes=["zero"])
    C.ts("dve", gsL[:], vec[:, 64:96], 1.0, ALU.add, ["vec"], ["vec"])
    C.tt("dve", gsL[:], gsL[:], vec[:, 0:32], ALU.mult, ["vec"], ["vec"])
    C.ts("dve", gsC[:], vec[:, 128:160], 1.0, ALU.add, ["vec"], ["vec"])
    C.tt("dve", gsC[:], gsC[:], vec[:, 0:32], ALU.mult, ["vec"], ["vec"])
    for k8 in range(16):
        C.castdma(wbf[k8 * 256:(k8 + 1) * 256, :], w_in[k8 * 256:(k8 + 1) * 256, :], ["wbf"])
    wbf_fm = fm(wbf)
    C.store(Zc[:, 0:1].rearrange("(c p) t -> p c t", p=128), zero[:, 0:1].unsqueeze(1).to_broadcast([128, 8, 1]), ["zero"], ["Zc"])
    C.store(Zc[:, LC + 1:LC + 2].rearrange("(c p) t -> p c t", p=128), zero[:, 0:1].unsqueeze(1).to_broadcast([128, 8, 1]), ["zero"], ["Zc"])

    tiles = [("HL", 0, 128, "halo"), ("L0", 128, 512, "lat"), ("L1", 640, 512, "lat"),
             ("L2", 1152, 512, "lat"), ("L3", 1664, 512, "lat"), ("HR", 2176, 128, "halo"), ("C", 0, LC, "ctx")]
    wl = 0
    pa = 0
    s16 = 0
    for name, t0, T, kind in tiles:
        isctx = kind == "ctx"
        src = fm(xcT) if isctx else fm(xT)
        rms_modulate(C, src, t0, T, xs, hT, sq, ssq, rstd, tmp, ones,
                     gsC if isctx else gsL, vec[:, 96:128] if isctx else vec[:, 32:64])
        if dbg and isctx:
            C.store(dbg_h, hT[:, :, 0:256], ["hT"], ["dbgh"])
            C.store(dbg_r, rstd[:, 0:256], ["rstd"], ["dbgr"])
        if kind == "lat" or kind == "halo":
            C.load(rc[:, 0:T], ropec[:, t0:t0 + T], ["rc"])
            C.load(rs[:, 0:T], ropes[:, t0:t0 + T], ["rs"])
        if kind == "lat" or (isctx and not last):
            groups = list(range(14))
        elif kind == "halo":
            groups = [4, 5, 8, 9, 10, 11]
        else:
            groups = [4, 5]
        for g in groups:
            ws = wl % 2
            wl += 1
            C.load(wt[ws][:], wbf_fm[:, :, g * 512:(g + 1) * 512], [("wt", ws)], ["wbf"])
            if dbg and isctx and g == 5:
                C.store(dbg_w, wt[ws][:], [("wt", ws)], ["dbgw"])
            if g == 5:
                for tb in range(T // 128):
                    p = pacc[pa % 3]; pk = ("pacc", pa % 3); pa += 1
                    for kc in range(32):
                        C.mm(p[:], hT[:, kc, tb * 128:(tb + 1) * 128], wt[ws][:, kc, :], kc == 0, kc == 31,
                             ["hT", ("wt", ws)], [pk])
                    sb_ = st16[s16 % 3]; sk = ("st16", s16 % 3); s16 += 1
                    C.copy("act", sb_[:], p[:], [pk], [sk])
                    dst = vcO[tb * 128:(tb + 1) * 128, :] if isctx else vO[t0 + tb * 128:t0 + (tb + 1) * 128, :]
                    C.store(dst, sb_[:], [sk], ["vout"])
                continue
            for j in range(4):
                n = 4 * g + j
                p = pacc[pa % 3]; pk = ("pacc", pa % 3); pa += 1
                for kc in range(32):
                    C.mm(p[:, 0:T], wt[ws][:, kc, j * 128:(j + 1) * 128], hT[:, kc, 0:T], kc == 0, kc == 31,
                         ["hT", ("wt", ws)], [pk])
                if g <= 4:
                    sb_ = st16[s16 % 3]; sk = ("st16", s16 % 3); s16 += 1
                    if isctx:
                        C.copy("act", sb_[:, 0:T], p[:, 0:T], [pk], [sk])
                        dst = qcT[n * 128:(n + 1) * 128, :] if g < 4 else kcT[(n - 16) * 128:(n - 15) * 128, :]
                    else:
                        xi = pa % 2
                        C.copy("act", xq[xi][:, 0:T], p[:, 0:T], [pk], [("xq", xi)])
                        C.mm(pperm[xi][:, 0:T], perm[:], xq[xi][:, 0:T], True, True, ["perm", ("xq", xi)], [("pperm", xi)])
                        C.tt("dve", tmp[xi][:, 0:T], pperm[xi][:, 0:T], rs[:, 0:T], ALU.mult, [("pperm", xi), "rs"], [("tmp", xi)])
                        C.tt("pool", xq[xi][:, 0:T], xq[xi][:, 0:T], rc[:, 0:T], ALU.mult, [("xq", xi), "rc"], [("xq", xi)])
                        C.tt("dve", sb_[:, 0:T], xq[xi][:, 0:T], tmp[xi][:, 0:T], ALU.add, [("xq", xi), ("tmp", xi)], [sk])
                        if g < 4:
                            dst = qT[n * 128:(n + 1) * 128, t0 - 128:t0 - 128 + T]
                        else:
                            dst = kT[(n - 16) * 128:(n - 15) * 128, t0:t0 + T]
                    C.store(dst, sb_[:, 0:T], [sk], ["qkout"])
                elif g in (6, 7):
                    sb_ = st16[s16 % 3]; sk = ("st16", s16 % 3); s16 += 1
                    C.copy("act", sb_[:, 0:T], p[:, 0:T], [pk], [sk])
                    c8 = n - 24
                    dst = BGc[c8 * 128:(c8 + 1) * 128, :] if isctx else BG[c8 * 128:(c8 + 1) * 128, t0:t0 + T]
                    C.store(dst, sb_[:, 0:T], [sk], ["BG"])
                elif g in (8, 9):
                    C.copy("act", cgs[:, n - 32, 0:T], p[:, 0:T], [pk], ["cgs"])
                elif g in (10, 11):
                    c8 = n - 40
                    zi = pa % 2
                    zt = st32[zi]; zk = ("st32", zi)
                    if kind == "halo":
                        fl = vec[:, 184:185] if name == "HL" else vec[:, 185:186]
                        C.stt("dve", zt[:, 0:T], cgs[:, c8, 0:T], fl, p[:, 0:T], ALU.mult, ALU.mult, ["cgs", pk, "vec"], [zk])
                    else:
                        C.tt("dve", zt[:, 0:T], cgs[:, c8, 0:T], p[:, 0:T], ALU.mult, ["cgs", pk], [zk])
                    dst = Zc[c8 * 128:(c8 + 1) * 128, 1:1 + LC] if isctx else Z[c8 * 128:(c8 + 1) * 128, t0:t0 + T]
                    C.store(dst, zt[:, 0:T], [zk], ["Z"])
                else:
                    C.copy("act", fuT[:, n - 48, 0:T], p[:, 0:T], [pk], ["fuT"])
        if kind == "lat" or (isctx and not last):
            for tb in range(T // 128):
                for gi in range(4):
                    p = pacc[pa % 3]; pk = ("pacc", pa % 3); pa += 1
                    for cs in range(2):
                        for kc in range(2):
                            C.mm(p[:, cs * 256:(cs + 1) * 256], fuT[:, gi * 2 + kc, tb * 128:(tb + 1) * 128], dft[:, kc, cs, :],
                                 cs == 0 and kc == 0, cs == 1 and kc == 1, ["fuT", "dft"], [pk])
                    if isctx:
                        C.copy("act", abc[:, tb, :, gi * 256:(gi + 1) * 256], p[:].rearrange("p (a c) -> p a c", a=2), [pk], ["abc"])
                    else:
                        sb_ = st16[s16 % 3]; sk = ("st16", s16 % 3); s16 += 1
                        C.copy("act", sb_[:], p[:], [pk], [sk])
                        tt0 = t0 - 128 + tb * 128
                        C.store(AB[:, tt0:tt0 + 128, gi * 256:(gi + 1) * 256].rearrange("a t c -> t a c"),
                                sb_[:].rearrange("p (a c) -> p a c", a=2), [sk], ["AB"])
            if isctx:
                for c8 in range(8):
                    p = pacc[pa % 3]; pk = ("pacc", pa % 3); pa += 1
                    i = 0
                    for ab, var in ((0, 0), (1, 2)):
                        for tb in range(2):
                            C.mm(p[:, 0:LC], abc[:, tb, ab, c8 * 128:(c8 + 1) * 128], dft[:, tb, var, :], i == 0, i == 3,
                                 ["abc", "dft"], [pk])
                            i += 1
                    sb_ = st16[s16 % 3]; sk = ("st16", s16 % 3); s16 += 1
                    C.act(sb_[:, 0:LC], p[:, 0:LC], AF.Copy, [pk], [sk], scale=1.0 / 256.0)
                    C.store(fourcT[c8 * 128:(c8 + 1) * 128, :], sb_[:, 0:LC], [sk], ["fourc"])
    jobs = [(Z, BG, convT, 128 + 512 * i, 512, 512 * i) for i in range(4)]
    if not last:
        jobs.append((Zc, BGc, convcT, 1, LC, 0))
    k = 0
    for Zs, BGs, dstT, z0, T, o0 in jobs:
        for c8 in range(8):
            zi = k % 2; k += 1
            zt = st32[zi]; zk = ("st32", zi)
            C.load(zt[:, 0:T + 2], Zs[c8 * 128:(c8 + 1) * 128, z0 - 1:z0 + T + 1], [zk], ["Z"])
            bgt = st16[s16 % 3]; bk = ("st16", s16 % 3); s16 += 1
            bo = z0 if Zs is Z else 0
            C.load(bgt[:, 0:T], BGs[c8 * 128:(c8 + 1) * 128, bo:bo + T], [bk], ["BG"])
            y = tmp[zi]; yk = ("tmp", zi)
            C.ts("dve", y[:, 0:T], zt[:, 0:T], vec[:, 160 + c8:161 + c8], ALU.mult, [zk, "vec"], [yk])
            C.stt("dve", y[:, 0:T], zt[:, 1:T + 1], vec[:, 168 + c8:169 + c8], y[:, 0:T], ALU.mult, ALU.add, [zk, "vec", yk], [yk])
            C.stt("dve", y[:, 0:T], zt[:, 2:T + 2], vec[:, 176 + c8:177 + c8], y[:, 0:T], ALU.mult, ALU.add, [zk, "vec", yk], [yk])
            ob = st16[s16 % 3]; ok = ("st16", s16 % 3); s16 += 1
            C.tt("pool", ob[:, 0:T], y[:, 0:T], bgt[:, 0:T], ALU.mult, [yk, bk], [ok])
            C.store(dstT[c8 * 128:(c8 + 1) * 128, o0:o0 + T], ob[:, 0:T], [ok], ["convout"])
    S.emit()
    return C.nc


def fmvec(v):
    v = np.asarray(v, np.float32)
    return np.ascontiguousarray(v.reshape(-1, 128).T)


def rope_tables():
    inv = 10000.0 ** (-np.arange(0, 64, 2, dtype=np.float64) / 64.0)
    t = np.arange(SEQ)
    row, col = (t // 64).astype(np.float64), (t % 64).astype(np.float64)
    d = np.arange(128)
    axis, within = d // 64, d % 64
    ph, f = within // 32, within % 32
    pos = np.where(axis[:, None] == 0, row[None, :], col[None, :])
    ang = (pos * inv[f][:, None]).astype(np.float32).astype(np.float64)
    cos = np.cos(ang).astype(np.float32)
    sin = (np.sin(ang) * np.where(ph == 0, -1.0, 1.0)[:, None]).astype(np.float32)
    perm = np.zeros((128, 128), np.float32)
    partner = np.where(ph == 0, d + 32, d - 32)
    perm[partner, d] = 1.0
    return cos, sin, perm


def dft256_table():
    k = np.arange(256, dtype=np.float64)
    ang = 2 * np.pi * np.outer(k, k) / 256.0
    t = np.stack([np.cos(ang), np.sin(ang), -np.sin(ang)], axis=1)
    return np.ascontiguousarray(t.reshape(2, 128, 3, 256).transpose(1, 0, 2, 3)).astype(NPBF)


def ext_slices(x2d):
    xp = np.zeros((SEQ + 256, x2d.shape[1]), x2d.dtype)
    xp[128:128 + SEQ] = x2d
    return [np.ascontiguousarray(xp[j * TL:j * TL + NEXT].T) for j in range(NCORES)]


def make_vec(j, g, shiftL, scaleL, shiftC, scaleC, conv_w):
    v = np.zeros((128, 192), np.float32)
    v[:, 0:32] = fmvec(g)
    v[:, 32:64] = fmvec(shiftL)
    v[:, 64:96] = fmvec(scaleL)
    v[:, 96:128] = fmvec(shiftC)
    v[:, 128:160] = fmvec(scaleC)
    for jj in range(3):
        v[:, 160 + jj * 8:168 + jj * 8] = fmvec(conv_w[jj])
    v[:, 184] = 0.0 if j == 0 else 1.0
    v[:, 185] = 0.0 if j == NCORES - 1 else 1.0
    return v


def build_L2():
    C = Ctx()
    S = C.S
    Ain = C.din("Ain", [SEQ, 128], BF16)
    Bin = C.din("Bin", [SEQ, 128], BF16)
    Wd = C.din("W", [128, 4, 128], BF16)
    twd = C.din("tw", [128, 2, 128])
    yT = C.dout("yT", [128, SEQ], BF16)
    ZS = C.dscr("ZS", [2, 128, 128, 128], BF16)
    XA = C.sb("XA", [128, SEQ], BF16)
    XB = C.sb("XB", [128, SEQ], BF16)
    ZT = [C.sb(f"ZT{i}", [128, 128, 128], BF16) for i in range(2)]
    W = C.sb("W", [128, 4, 128], BF16)
    tw = C.sb("tw", [128, 2, 128])
    ta = [C.sb(f"ta{i}", [128, 512]) for i in range(2)]
    tb_ = [C.sb(f"tb{i}", [128, 512]) for i in range(2)]
    zo = [[C.sb(f"zo{i}{x}", [128, 512], BF16) for x in range(2)] for i in range(2)]
    pz = [[C.ps(f"pz{i}{x}", [128, 512]) for x in range(2)] for i in range(2)]
    pc = [C.ps(f"pc{i}", [128, 512]) for i in range(2)]
    C.load(W[:], Wd, ["W"])
    C.load(tw[:], twd, ["tw"])
    for h in range(4):
        sl = slice(h * 4096, (h + 1) * 4096)
        C.load(XA[:, sl], Ain.rearrange("(a b) c -> a (b c)", a=128)[:, sl], [("XA", h)])
        C.load(XB[:, sl], Bin.rearrange("(a b) c -> a (b c)", a=128)[:, sl], [("XB", h)], q="pool")
    for g in range(32):
        i = g % 2
        cs = slice(g * 512, (g + 1) * 512)
        h = g // 8
        C.mm(pz[i][0][:], W[:, 0, :], XA[:, cs], True, False, ["W", ("XA", h)], [("pz", i, 0)])
        C.mm(pz[i][0][:], W[:, 2, :], XB[:, cs], False, True, ["W", ("XB", h)], [("pz", i, 0)])
        C.mm(pz[i][1][:], W[:, 2, :], XA[:, cs], True, False, ["W", ("XA", h)], [("pz", i, 1)])
        C.mm(pz[i][1][:], W[:, 3, :], XB[:, cs], False, True, ["W", ("XB", h)], [("pz", i, 1)])
        for k in range(4):
            t2 = g * 4 + k
            ks = slice(k * 128, (k + 1) * 128)
            C.act(ta[i][:, ks], pz[i][0][:, ks], AF.Copy, [("pz", i, 0), "tw"], [("ta", i)], scale=tw[:, 0, t2:t2 + 1])
            C.act(tb_[i][:, ks], pz[i][0][:, ks], AF.Copy, [("pz", i, 0), "tw"], [("tb", i)], scale=tw[:, 1, t2:t2 + 1])
        for k in range(4):
            t2 = g * 4 + k
            ks = slice(k * 128, (k + 1) * 128)
            C.stt("dve", zo[i][0][:, ks], pz[i][1][:, ks], tw[:, 1, t2:t2 + 1], ta[i][:, ks], ALU.mult, ALU.add,
                  [("pz", i, 1), "tw", ("ta", i)], [("zo", i, 0)])
            C.stt("dve", zo[i][1][:, ks], pz[i][1][:, ks], tw[:, 0, t2:t2 + 1], tb_[i][:, ks], ALU.mult, ALU.subtract,
                  [("pz", i, 1), "tw", ("tb", i)], [("zo", i, 1)])
        for x in range(2):
            C.store(ZS[x, :, g * 4:(g + 1) * 4, :], zo[i][x][:].rearrange("p (a c) -> p a c", a=4), [("zo", i, x)], ["ZS"],
                    q="sp" if x == 0 else "pool")
    for x in range(2):
        for h in range(4):
            C.load(ZT[x][:, h * 32:(h + 1) * 32, :], ZS[x, h * 32:(h + 1) * 32, :, :].rearrange("f t c -> t f c"),
                   [("ZT", x, h)], ["ZS"], q="sp" if x == 0 else "pool")
    Y = XA
    Y3 = Y[:].rearrange("p (f2 f1) -> p f2 f1", f1=128)
    for fg in range(32):
        i = fg % 2
        for k in range(4):
            f1 = fg * 4 + k
            ks = slice(k * 128, (k + 1) * 128)
            C.mm(pc[i][:, ks], ZT[0][:, f1, :], W[:, 0, :], k == 0, False, ["W", ("ZT", 0, f1 // 32)], [("pc", i)])
            C.mm(pc[i][:, ks], ZT[1][:, f1, :], W[:, 1, :], False, k == 3, ["W", ("ZT", 1, f1 // 32)], [("pc", i)])
        C.act(Y3[:, :, fg * 4:(fg + 1) * 4], pc[i][:].rearrange("p (a f) -> p f a", a=4), AF.Copy, [("pc", i)],
              [("XA", 0), ("XA", 1), ("XA", 2), ("XA", 3)], scale=1.0 / 2048.0)
    for h in range(4):
        sl = slice(h * 4096, (h + 1) * 4096)
        C.store(yT[:, sl], Y[:, sl], [("XA", h)], ["yT"], q="sp" if h % 2 == 0 else "pool")
    S.emit()
    return C.nc


def fft_tables():
    k = np.arange(128, dtype=np.float64)
    ang = 2 * np.pi * np.outer(k, k) / 128.0
    W = np.stack([np.cos(ang), np.sin(ang), -np.sin(ang), -np.cos(ang)], axis=1).astype(NPBF)
    ang2 = 2 * np.pi * np.outer(k, k) / float(SEQ)
    tw = np.stack([np.cos(ang2), np.sin(ang2)], axis=1).astype(np.float32)
    return np.ascontiguousarray(W), np.ascontiguousarray(tw)


def build_L3(last, NEe=NE, dbg=False):
    C = Ctx()
    S = C.S
    if dbg:
        dbg_mix = C.dout("dbg_mix", [128, 32, 512], BF16)
        dbg_xm = C.dout("dbg_xm", [128, 32, 512])
        dbg_wt = C.dout("dbg_wt", [128, 8192], BF16)
        dbg_wd = C.dout("dbg_wd", [128, 1536], BF16)
    qT = C.din("qT", [2048, TL], BF16)
    kT = C.din("kT", [512, NEXT], BF16)
    vI = C.din("v", [NEXT, 512], BF16)
    kcT = C.din("kcT", [512, LC], BF16)
    vcI = C.din("vc", [LC, 512], BF16)
    convT = C.din("convT", [1024, TL], BF16)
    fourT = C.din("fourT", [1024, TL], BF16)
    xT = C.din("xT", [D, TL])
    if not last:
        qcT = C.din("qcT", [2048, LC], BF16)
        convcT = C.din("convcT", [1024, LC], BF16)
        fourcT = C.din("fourcT", [1024, LC], BF16)
        xcT = C.din("xcT", [D, LC])
        xocT = C.dout("xocT", [D, LC])
    vecd = C.din("vec2", [128, 336])
    maskd = C.din("masks", [128, 4, 512], BF16)
    w_out = C.din("w_out", [D, D])
    wrd = C.din("wr", [128, 32, 32])
    brd = C.din("br", [128, 32])
    w_gu = C.din("w_gu", [NEe, 6, 128, 8192])
    bgud = C.din("bgu", [128, NEe, 12])
    w_dn = C.din("w_dn", [NEe, 128, 24576])
    bdnd = C.din("bdnT", [128, 32, NEe])
    identd = C.din("ident", [128, 128])
    xoT = C.dout("xoT", [D, TL])
    wobf = C.dscr("wobf", [D, D], BF16)
    wgubf = [C.dscr(f"wgubf{i}", [6, 128, 8192], BF16) for i in range(NEe)]
    wdnbf = [C.dscr(f"wdnbf{i}", [128, 24576], BF16) for i in range(NEe)]
    XM = C.dscr("XM", [D, 512])
    GT = C.dscr("GT", [32, 512])

    xt = C.sb("xt", [128, 32, 512])
    mix = C.sb("mix", [128, 32, 512], BF16)
    wt = [C.sb(f"wt{i}", [128, 32, 256], BF16) for i in range(2)]
    wdn = [C.sb(f"wdn{i}", [128, 6, 256], BF16) for i in range(2)]
    actT = C.sb("actT", [128, 6, 512], BF16)
    vec = C.sb("vec", [128, 336])
    gs2L = C.sb("gs2L", [128, 32])
    gs2C = C.sb("gs2C", [128, 32])
    masks = C.sb("masks", [128, 4, 512], BF16)
    wr = C.sb("wr", [128, 32, 32])
    br = C.sb("br", [128, 32])
    bgu = C.sb("bgu", [128, NEe, 12])
    bdn = C.sb("bdnT", [128, 32, NEe])
    ident = C.sb("ident", [128, 128])
    ones = C.sb("ones", [128, 128], BF16)
    zeros = C.sb("zeros", [128, 128])
    sinkrow = C.sb("sinkrow", [128, 16, 128])
    Kc = C.sb("Kc", [128, 4, LC], BF16)
    Vc = C.sb("Vc", [128, 2, 512], BF16)
    Kt = [C.sb(f"Kt{i}", [128, 768], BF16) for i in range(2)]
    Vt = [C.sb(f"Vt{i}", [128, 6, 128], BF16) for i in range(2)]
    Qt0 = C.sb("Qt0", [128, 4, 512], BF16)
    Qt = [Qt0, Qt0]
    Ej = [C.sb(f"E{i}", [128, 512], BF16) for i in range(4)]
    sq = [C.sb(f"sq{i}", [128, 512], BF16) for i in range(2)]
    tmp = [C.sb(f"tmp{i}", [128, 512]) for i in range(4)]
    rstd = C.sb("rstd", [128, 512])
    den = C.sb("den", [128, 512])
    gbs2 = [C.sb(f"gbs{i}", [128, 512]) for i in range(2)]
    gT = C.sb("gT", [32, 512])
    rt = C.sb("rt", [128, 96])
    rt2 = C.sb("rt2", [128, 16])
    bank = [C.ps(f"bank{i}", [128, 512]) for i in range(8)]
    bk = lambda i: ("bank", i)

    C.load(vec[:], vecd, ["vec"])
    C.load(masks[:], maskd, ["masks"])
    C.load(wr[:], wrd, ["wr"])
    C.load(br[:], brd, ["br"])
    C.load(bgu[:], bgud, ["bgu"])
    C.load(bdn[:], bdnd, ["bdn"])
    C.load(ident[:], identd, ["ident"])
    C.load(Kc[:], kcT.rearrange("(h p) t -> p h t", p=128), ["Kc"])
    C.load(Vc[:], vcI.rearrange("(b p) d -> p b d", p=128), ["Vc"])
    S.op("pool", lambda e: e.memset(ones[:], 1.0), writes=["ones"])
    S.op("pool", lambda e: e.memset(zeros[:], 0.0), writes=["zeros"])
    for h in range(16):
        C.act(sinkrow[:, h, :], zeros[:], AF.Exp, ["zeros", "vec"], ["sinkrow"], bias=vec[:, 320 + h:321 + h])
    C.ts("dve", gs2L[:], vec[:, 96:128], 1.0, ALU.add, ["vec"], ["gs2"])
    C.tt("dve", gs2L[:], gs2L[:], vec[:, 32:64], ALU.mult, ["vec", "gs2"], ["gs2"])
    C.ts("dve", gs2C[:], vec[:, 224:256], 1.0, ALU.add, ["vec"], ["gs2"])
    C.tt("dve", gs2C[:], gs2C[:], vec[:, 32:64], ALU.mult, ["vec", "gs2"], ["gs2"])

    for k8 in range(8):
        C.castdma(wobf[k8 * 512:(k8 + 1) * 512, :], w_out[k8 * 512:(k8 + 1) * 512, :], ["wbfo"])
    for e_ in range(NEe):
        for fc in range(6):
            C.castdma(wgubf[e_][fc], w_gu[e_, fc], [("wbfe", e_)])
        for q4 in range(6):
            C.castdma(wdnbf[e_][:, q4 * 4096:(q4 + 1) * 4096], w_dn[e_, :, q4 * 4096:(q4 + 1) * 4096], [("wbfe", e_)])
    allxt = [("xt", c) for c in range(32)]

    wobf_fm = fm(wobf)
    tiles = [(f"L{i}", 512 * i, 512, "lat") for i in range(4)]
    if not last:
        tiles.append(("C", 0, LC, "ctx"))
    wl = 0
    pa = 0
    ei = 0
    qi = 0
    ti = 0
    wdl = 0
    for name, t0, T, kind in tiles:
        isctx = kind == "ctx"
        pe2 = "dve" if name == "L0" else "pool"
        xsrc = fm(xcT) if isctx else fm(xT)
        xdst = fm(xocT) if isctx else fm(xoT)
        o_ = 160 if isctx else 0
        gmix = vec[:, 160:192] if isctx else vec[:, 0:32]
        shift2 = vec[:, 192:224] if isctx else vec[:, 64:96]
        gffn = vec[:, 256:288] if isctx else vec[:, 128:160]
        gs2 = gs2C if isctx else gs2L
        for cgi in range(4):
            C.load(xt[:, cgi * 8:(cgi + 1) * 8, 0:T], xsrc[:, cgi * 8:(cgi + 1) * 8, t0:t0 + T],
                   [("xt", c) for c in range(cgi * 8, cgi * 8 + 8)])
        for kvh in range(4):
            ks_ = (kvh + (0 if isctx else 0)) % 2
            if not isctx:
                C.load(Kt[ks_][:], kT[kvh * 128:(kvh + 1) * 128, t0:t0 + 768], [("Kt", ks_)])
                C.load(Vt[ks_][:], vI[t0:t0 + 768, kvh * 128:(kvh + 1) * 128].rearrange("(b p) d -> p b d", p=128), [("Vt", ks_)])
            qsrc = qcT if isctx else qT
            C.load(Qt[ks_][:, :, 0:T], qsrc[kvh * 512:(kvh + 1) * 512, t0:t0 + T].rearrange("(g p) t -> p g t", p=128), [("Qt", 0)])
            for b in range(T // 128):
                blocks = [("ctx", 0), ("ctx", 1)] if isctx else [("loc", 0), ("loc", 1), ("loc", 2), ("ctx", 0), ("ctx", 1)]
                pO = 3 + (qi % 2)
                pD = 5 + (qi % 2)
                qi += 1
                for bi, (bt, jb) in enumerate(blocks):
                    pS = pa % 3
                    pa += 1
                    if bt == "loc":
                        lk = Kt[ks_][:, (b + jb) * 128:(b + jb + 1) * 128]
                        lv = Vt[ks_][:, b + jb, :]
                        rk = [("Kt", ks_)]
                        rv = [("Vt", ks_)]
                    else:
                        lk = Kc[:, kvh, jb * 128:(jb + 1) * 128]
                        lv = Vc[:, jb, kvh * 128:(kvh + 1) * 128]
                        rk = ["Kc"]
                        rv = ["Vc"]
                    C.mm(bank[pS][:].rearrange("p (g q) -> p g q", g=4), lk, Qt[ks_][:, :, b * 128:(b + 1) * 128], True, True,
                         rk + [("Qt", 0)], [bk(pS)])
                    E = Ej[ei % 4]
                    ek = ("E", ei % 4)
                    ei += 1
                    C.act(E[:], bank[pS][:], AF.Exp, [bk(pS)], [ek], scale=float(128 ** -0.5))
                    if bt == "loc" and jb != 1:
                        if jb == 0:
                            mi = 0 if (name == "L0" and b == 0) else 1
                        else:
                            mi = 3 if (name == "L3" and b == 3) else 2
                        C.tt("dve", E[:], E[:], masks[:, mi, :], ALU.mult, [ek, "masks"], [ek])
                    C.mm(bank[pO][:], lv, E[:], bi == 0, bi == len(blocks) - 1, rv + [ek], [bk(pO)])
                    C.mm(bank[pD][:], ones[:], E[:], bi == 0, bi == len(blocks) - 1, ["ones", ek], [bk(pD)])
                C.tt("dve", den[:], bank[pD][:], sinkrow[:, kvh * 4:(kvh + 1) * 4, :].rearrange("p g q -> p (g q)"), ALU.add,
                     [bk(pD), "sinkrow"], ["den"])
                S.op("dve", lambda e: e.reciprocal(out=den[:], in_=den[:]), reads=["den"], writes=["den"])
                C.tt("dve", mix[:, kvh * 4:(kvh + 1) * 4, b * 128:(b + 1) * 128], bank[pO][:].rearrange("p (g q) -> p g q", g=4),
                     den[:].rearrange("p (g q) -> p g q", g=4), ALU.mult, [bk(pO), "den"], ["mix"])
        csrc, fsrc = (convcT, fourcT) if isctx else (convT, fourT)
        C.load(mix[:, 16:24, 0:T], fm(csrc)[:, :, t0:t0 + T], ["mix"])
        C.load(mix[:, 24:32, 0:T], fm(fsrc)[:, :, t0:t0 + T], ["mix"])
        if dbg and name == "L0":
            C.store(dbg_mix, mix[:], ["mix"], ["dbgmix"])
        for g in range(16):
            ws = wl % 2
            wl += 1
            C.load(wt[ws][:], wobf_fm[:, :, g * 256:(g + 1) * 256], [("wt", ws)], ["wbfo"])
            for j in range(2):
                n = 2 * g + j
                p = pa % 3
                pa += 1
                for kc in range(32):
                    C.mm(bank[p][:, 0:T], wt[ws][:, kc, j * 128:(j + 1) * 128], mix[:, kc, 0:T], kc == 0, kc == 31,
                         ["mix", ("wt", ws)], [bk(p)])
                C.stt("dve", xt[:, n, 0:T], bank[p][:, 0:T], gmix[:, n:n + 1], xt[:, n, 0:T], ALU.mult, ALU.add,
                      [bk(p), "vec", ("xt", n)], [("xt", n)])
        if dbg and name == "L0":
            C.store(dbg_xm, xt[:], allxt, ["dbgxm"])
        for c in range(32):
            q = c % 2
            C.act(sq[q][:, 0:T], xt[:, c, 0:T], AF.Square, [("xt", c)], [("sq", q)])
            C.mm(bank[7][:, 0:T], ones[:], sq[q][:, 0:T], c == 0, c == 31, [("sq", q), "ones"], [bk(7)])
        C.ts("dve", rstd[:, 0:T], bank[7][:, 0:T], 1.0 / D, ALU.mult, [bk(7)], ["rstd"], s2=EPS, op1=ALU.add)
        C.act(rstd[:, 0:T], rstd[:, 0:T], AF.Sqrt, ["rstd"], ["rstd"])
        S.op("dve", lambda e, T=T: e.reciprocal(out=rstd[:, 0:T], in_=rstd[:, 0:T]), reads=["rstd"], writes=["rstd"])
        nb = T // 128
        for c in range(32):
            q = ti % 2
            q2 = 2 + ti % 2
            ti += 1
            C.tt("dve", tmp[q][:, 0:T], xt[:, c, 0:T], rstd[:, 0:T], ALU.mult, [("xt", c), "rstd"], [("tmp", q)])
            C.act(tmp[q2][:, 0:T], tmp[q][:, 0:T], AF.Identity, [("tmp", q), "vec", "gs2"], [("tmp", q2)],
                  bias=shift2[:, c:c + 1], scale=gs2[:, c:c + 1])
            C.copy(pe2, mix[:, c, 0:T], tmp[q2][:, 0:T], [("tmp", q2)], ["mix"])
            for tb in range(nb):
                C.mm(bank[3 + tb][:, 0:32], tmp[q2][:, tb * 128:(tb + 1) * 128], wr[:, c, :],
                     c == 0, c == 31, [("tmp", q2), "wr"], [bk(3 + tb)])
        for cgi in range(4):
            C.store(fm(XM)[:, cgi * 8:(cgi + 1) * 8, 0:T], xt[:, cgi * 8:(cgi + 1) * 8, 0:T],
                    [("xt", c) for c in range(cgi * 8, cgi * 8 + 8)], [("XM", cgi)], q="sp")
        for tb in range(nb):
            C.tt("dve", rt[:, 0:32], bank[3 + tb][:, 0:32], br[:], ALU.add, [bk(3 + tb), "br"], ["rt"])
            S.op("dve", lambda e: e.max(out=rt2[:, 0:8], in_=rt[:, 0:32]), reads=["rt"], writes=["rt2"])
            C.ts("dve", rt[:, 64:96], rt[:, 0:32], rt2[:, 3:4], ALU.is_ge, ["rt", "rt2"], ["rtm"])
            C.ts("dve", rt2[:, 8:9], rt2[:, 0:1], -1.0, ALU.mult, ["rt2"], ["rt2n"])
            C.act(rt[:, 32:64], rt[:, 0:32], AF.Exp, ["rt", "rt2n"], ["rte"], bias=rt2[:, 8:9])
            C.tt("dve", rt[:, 32:64], rt[:, 32:64], rt[:, 64:96], ALU.mult, ["rte", "rtm"], ["rte"])
            S.op("dve", lambda e: e.reduce_sum(out=rt2[:, 9:10], in_=rt[:, 32:64], axis=mybir.AxisListType.X),
                 reads=["rte"], writes=["rt2s"])
            S.op("dve", lambda e: e.reciprocal(out=rt2[:, 10:11], in_=rt2[:, 9:10]), reads=["rt2s"], writes=["rt2r"])
            C.ts("dve", rt[:, 32:64], rt[:, 32:64], rt2[:, 10:11], ALU.mult, ["rte", "rt2r"], ["rte"])
            gb_ = (0, 1, 2, 7)[tb]
            C.mm(bank[gb_][0:32, 0:128], rt[:, 32:64], ident[:], True, True, ["rte", "ident"], [bk(gb_)])
            C.copy("act", gT[:, tb * 128:(tb + 1) * 128], bank[gb_][0:32, 0:128], [bk(gb_)], ["gT"])
        C.store(GT[:, 0:T], gT[:, 0:T], ["gT"], ["GT"], q="sp")
        S.op(pe2, lambda e, T=T: e.memset(xt[:, :, 0:T], 0.0), reads=[("XM", i) for i in range(4)], writes=allxt)
        for e_ in range(NEe):
            gbs = gbs2[e_ % 2]
            gk = ("gbs", e_ % 2)
            for hh in range(T // 256):
                C.load(gbs[:, hh * 256:(hh + 1) * 256], GT[e_:e_ + 1, hh * 256:(hh + 1) * 256].partition_broadcast(128), [gk], ["GT"])
            for n in range(32):
                C.stt("dve", xt[:, n, 0:T], gbs[:, 0:T], bdn[:, n, e_:e_ + 1], xt[:, n, 0:T], ALU.mult, ALU.add,
                      [gk, "bdn", ("xt", n)], [("xt", n)])
            for fc in range(6):
                ws = wl % 2
                wl += 1
                C.load(wt[ws][:].rearrange("p c t -> p (c t)"), wgubf[e_][fc], [("wt", ws)], [("wbfe", e_)])
                wv = wt[ws][:].rearrange("p c t -> p (c t)").rearrange("p (g k j) -> p g k j", g=2, k=32)
                if dbg and name == "L0" and e_ == 1 and fc == 2:
                    C.store(dbg_wt, wt[ws][:].rearrange("p c t -> p (c t)"), [("wt", ws)], ["dbgwt"])
                pgt = pa % 8
                pa += 1
                put = pa % 8
                pa += 1
                for kc in range(32):
                    C.mm(bank[pgt][:, 0:T], wv[:, 0, kc, :], mix[:, kc, 0:T], kc == 0, kc == 31, ["mix", ("wt", ws)], [bk(pgt)])
                for kc in range(32):
                    C.mm(bank[put][:, 0:T], wv[:, 1, kc, :], mix[:, kc, 0:T], kc == 0, kc == 31, ["mix", ("wt", ws)], [bk(put)])
                g1, sg, u1 = tmp[0], tmp[1], tmp[2]
                C.ts("dve", g1[:, 0:T], bank[pgt][:, 0:T], bgu[:, e_, fc:fc + 1], ALU.add, [bk(pgt), "bgu"], [("tmp", 0)], s2=7.0, op1=ALU.min)
                C.act(sg[:, 0:T], g1[:, 0:T], AF.Sigmoid, [("tmp", 0)], [("tmp", 1)], scale=1.702)
                C.ts("dve", u1[:, 0:T], bank[put][:, 0:T], bgu[:, e_, 6 + fc:7 + fc], ALU.add, [bk(put), "bgu"], [("tmp", 2)], s2=7.0, op1=ALU.min)
                C.ts(pe2, u1[:, 0:T], u1[:, 0:T], -7.0, ALU.max, [("tmp", 2)], [("tmp", 2)], s2=1.0, op1=ALU.add)
                C.tt(pe2, sg[:, 0:T], sg[:, 0:T], g1[:, 0:T], ALU.mult, [("tmp", 0), ("tmp", 1)], [("tmp", 1)])
                C.tt("dve", sg[:, 0:T], sg[:, 0:T], u1[:, 0:T], ALU.mult, [("tmp", 1), ("tmp", 2)], [("tmp", 1)])
                C.tt(pe2, actT[:, fc, 0:T], sg[:, 0:T], gbs[:, 0:T], ALU.mult, [("tmp", 1), gk], [("actT", fc)])
            for ng in range(16):
                ds_ = wdl % 2
                wdl += 1
                C.load(wdn[ds_][:].rearrange("p f j -> p (f j)"), wdnbf[e_][:, ng * 1536:(ng + 1) * 1536], [("wdn", ds_)], [("wbfe", e_)])
                if dbg and name == "L0" and e_ == 1 and ng == 3:
                    C.store(dbg_wd, wdn[ds_][:].rearrange("p f j -> p (f j)"), [("wdn", ds_)], ["dbgwd"])
                for j in range(2):
                    n = ng * 2 + j
                    p = pa % 8
                    pa += 1
                    for fc in range(6):
                        C.mm(bank[p][:, 0:T], wdn[ds_][:, fc, j * 128:(j + 1) * 128], actT[:, fc, 0:T], fc == 0, fc == 5,
                             [("wdn", ds_), ("actT", fc)], [bk(p)])
                    C.tt("dve", xt[:, n, 0:T], xt[:, n, 0:T], bank[p][:, 0:T], ALU.add, [bk(p), ("xt", n)], [("xt", n)])
        for n in range(32):
            q = ti % 4
            ti += 1
            C.load(tmp[q][:, 0:T], fm(XM)[:, n, 0:T], [("tmp", q)], [("XM", n // 8)])
            C.stt("dve", xt[:, n, 0:T], xt[:, n, 0:T], gffn[:, n:n + 1], tmp[q][:, 0:T], ALU.mult, ALU.add,
                  [("xt", n), "vec", ("tmp", q)], [("xt", n)])
        if not last:
            for cgi in range(4):
                C.store(xdst[:, cgi * 8:(cgi + 1) * 8, t0:t0 + T], xt[:, cgi * 8:(cgi + 1) * 8, 0:T],
                        [("xt", c) for c in range(cgi * 8, cgi * 8 + 8)], ["xout"], q="sp")
        else:
            for c in range(32):
                q = c % 2
                C.act(sq[q][:, 0:T], xt[:, c, 0:T], AF.Square, [("xt", c)], [("sq", q)])
                C.mm(bank[7][:, 0:T], ones[:], sq[q][:, 0:T], c == 0, c == 31, [("sq", q), "ones"], [bk(7)])
            C.ts("dve", rstd[:, 0:T], bank[7][:, 0:T], 1.0 / D, ALU.mult, [bk(7)], ["rstd"], s2=EPS, op1=ALU.add)
            C.act(rstd[:, 0:T], rstd[:, 0:T], AF.Sqrt, ["rstd"], ["rstd"])
            S.op("dve", lambda e, T=T: e.reciprocal(out=rstd[:, 0:T], in_=rstd[:, 0:T]), reads=["rstd"], writes=["rstd"])
            for c in range(32):
                q = ti % 4
                ti += 1
                C.tt("dve", tmp[q][:, 0:T], xt[:, c, 0:T], rstd[:, 0:T], ALU.mult, [("xt", c), "rstd"], [("tmp", q)])
                C.act(xt[:, c, 0:T], tmp[q][:, 0:T], AF.Copy, [("tmp", q), "vec"], [("xt", c)], scale=vec[:, 288 + c:289 + c])
            for cgi in range(4):
                C.store(xdst[:, cgi * 8:(cgi + 1) * 8, t0:t0 + T], xt[:, cgi * 8:(cgi + 1) * 8, 0:T],
                        [("xt", c) for c in range(cgi * 8, cgi * 8 + 8)], ["xout"], q="sp")
    S.emit()
    return C.nc


def make_vec2(modL, modC, g_ffn, g_final, sink):
    v = np.zeros((128, 336), np.float32)
    v[:, 0:32] = fmvec(modL[2])
    v[:, 32:64] = fmvec(g_ffn)
    v[:, 64:96] = fmvec(modL[3])
    v[:, 96:128] = fmvec(modL[4])
    v[:, 128:160] = fmvec(modL[5])
    v[:, 160:192] = fmvec(modC[2])
    v[:, 192:224] = fmvec(modC[3])
    v[:, 224:256] = fmvec(modC[4])
    v[:, 256:288] = fmvec(modC[5])
    v[:, 288:320] = fmvec(g_final)
    v[:, 320:336] = np.asarray(sink, np.float32)[None, :]
    return v


def make_masks(j):
    k = np.arange(128)[:, None]
    q = np.arange(128)[None, :]
    mp = np.tile((k >= q).astype(np.float32), (1, 4))
    mn = np.tile((k <= q).astype(np.float32), (1, 4))
    z = np.zeros_like(mp)
    m = np.stack([z if j == 0 else mp, mp, mn, z if j == NCORES - 1 else mn], axis=1)
    return np.ascontiguousarray(m).astype(NPBF)


def expert_layout(w_gu_l, w_dn_l):
    ne = w_gu_l.shape[0]
    a = w_gu_l.reshape(ne, 32, 128, 2, 6, 128).transpose(0, 4, 2, 3, 1, 5)
    a = np.ascontiguousarray(a).reshape(ne, 6, 128, 8192)
    b = w_dn_l.reshape(ne, 6, 128, 16, 256).transpose(0, 2, 3, 1, 4)
    b = np.ascontiguousarray(b).reshape(ne, 128, 24576)
    return a, b


def moe_small_inputs(w_router, b_router, b_gu, b_dn, ne=NE):
    wr = np.ascontiguousarray(np.asarray(w_router, np.float32).reshape(32, 128, 32).transpose(1, 0, 2))
    br = np.ascontiguousarray(np.broadcast_to(np.asarray(b_router, np.float32)[None, :], (128, 32)))
    bgu = np.ascontiguousarray(np.asarray(b_gu, np.float32)[:ne].reshape(ne, 12, 128).transpose(2, 0, 1))
    bdnT = np.ascontiguousarray(np.asarray(b_dn, np.float32)[:ne].reshape(ne, 32, 128).transpose(2, 1, 0))
    return wr, br, bgu, bdnT


def _run(nc, in_maps):
    return run_bass_kernel_spmd(nc, in_maps, core_ids=list(range(NCORES))).results


def ext_from_fm(xfm_list):
    full = np.concatenate(xfm_list, axis=1)
    pad = np.zeros((full.shape[0], SEQ + 256), full.dtype)
    pad[:, 128:128 + SEQ] = full
    return [np.ascontiguousarray(pad[:, j * TL:j * TL + NEXT]) for j in range(NCORES)]


def kernel(x, c, ctx, c_ctx, w_ada, b_ada, g_mix, w_in, conv_w, attn_sink, w_out, g_ffn,
           w_router, b_router, w_gu, b_gu, w_dn, b_dn, g_final):
    f = lambda a: np.asarray(a, np.float32)
    x, c, ctx, c_ctx = f(x), f(c), f(ctx), f(c_ctx)
    w_ada, b_ada, g_mix, w_in, conv_w, attn_sink = f(w_ada), f(b_ada), f(g_mix), f(w_in), f(conv_w), f(attn_sink)
    w_out, g_ffn, w_router, b_router = f(w_out), f(g_ffn), f(w_router), f(b_router)
    w_gu, b_gu, w_dn, b_dn, g_final = f(w_gu), f(b_gu), f(w_dn), f(b_dn), f(g_final)
    R = range(NCORES)
    cond = np.ascontiguousarray(np.stack([c[0], c_ctx], 0).reshape(2, 32, 128).transpose(2, 1, 0))
    res = _run(build_L0(), [{"cond": cond, "wada": np.ascontiguousarray(w_ada[:, :, j * NA:(j + 1) * NA]),
                             "bada": np.ascontiguousarray(np.broadcast_to(b_ada[:, None, j * NA:(j + 1) * NA], (2, 2, NA)))}
                            for j in R])
    mod = np.concatenate([res[j]["mod"] for j in R], axis=2).reshape(2, 2, 6, D)
    cos, sin, perm = rope_tables()
    cosp = np.zeros((128, SEQ + 256), np.float32)
    cosp[:, 128:128 + SEQ] = cos
    sinp = np.zeros((128, SEQ + 256), np.float32)
    sinp[:, 128:128 + SEQ] = sin
    dft = dft256_table()
    Wf, twf = fft_tables()
    ident = np.eye(128, dtype=np.float32)
    nc2 = build_L2()
    xfm = [np.ascontiguousarray(x[0, j * TL:(j + 1) * TL].T) for j in R]
    xcfm = np.ascontiguousarray(ctx[0].T)
    for l in range(2):
        last = l == 1
        modL, modC = mod[l, 0], mod[l, 1]
        xext = ext_from_fm(xfm)
        r1 = _run(build_L1(last), [{
            "xT": xext[j], "xcT": xcfm,
            "vec": make_vec(j, g_mix[l], modL[0], modL[1], modC[0], modC[1], conv_w[l]),
            "w_in": w_in[l], "ropec": np.ascontiguousarray(cosp[:, j * TL:j * TL + NEXT]),
            "ropes": np.ascontiguousarray(sinp[:, j * TL:j * TL + NEXT]), "perm": perm, "dft": dft} for j in R])
        del xext
        A = np.concatenate([r1[j]["AB"][0] for j in R], axis=0)
        B = np.concatenate([r1[j]["AB"][1] for j in R], axis=0)
        r2 = _run(nc2, [{"Ain": np.ascontiguousarray(A[:, j * 128:(j + 1) * 128]),
                         "Bin": np.ascontiguousarray(B[:, j * 128:(j + 1) * 128]), "W": Wf, "tw": twf} for j in R])
        four = np.concatenate([r2[j]["yT"] for j in R], axis=0)
        del A, B, r2
        wr, br, bgu, bdnT = moe_small_inputs(w_router[l], b_router[l], b_gu[l], b_dn[l])
        vec2 = make_vec2(modL, modC, g_ffn[l], g_final, attn_sink[l])
        wgu_l, wdn_l = expert_layout(w_gu[l], w_dn[l])
        maps = []
        for j in R:
            m = {"qT": r1[j]["qT"], "kT": r1[j]["kT"], "v": r1[j]["v"], "kcT": r1[j]["kcT"], "vc": r1[j]["vc"],
                 "convT": r1[j]["convT"], "fourT": np.ascontiguousarray(four[:, j * TL:(j + 1) * TL]), "xT": xfm[j],
                 "vec2": vec2, "masks": make_masks(j), "w_out": w_out[l], "wr": wr, "br": br, "w_gu": wgu_l,
                 "bgu": bgu, "w_dn": wdn_l, "bdnT": bdnT, "ident": ident}
            if not last:
                m.update({"qcT": r1[j]["qcT"], "convcT": r1[j]["convcT"], "fourcT": r1[j]["fourcT"], "xcT": xcfm})
            maps.append(m)
        r3 = _run(build_L3(last), maps)
        del maps, r1, four
        xfm = [r3[j]["xoT"] for j in R]
        if not last:
            xcfm = r3[0]["xocT"]
        del r3
    out = np.concatenate([xfm[j].T for j in R], axis=0)[None]
    return np.ascontiguousarray(out.astype(np.float32))
```

```python
import numpy as np
import ml_dtypes
import concourse.bass as bass
import concourse.mybir as mybir
from concourse.bass_utils import run_bass_kernel_spmd

F32 = mybir.dt.float32
BF16 = mybir.dt.bfloat16
AF = mybir.ActivationFunctionType
ALU = mybir.AluOpType
NPBF = ml_dtypes.bfloat16

NCORES = 8
D = 4096
SEQ = 16384
TL = SEQ // NCORES
NEXT = TL + 256
LC = 256
INW = 7168
NE = 32
DE = 768
EPS = 1e-5


class Sched:
    ENGS = ("pe", "act", "dve", "pool", "sp")
    NDMASEM = 8

    def __init__(self, nc):
        self.nc = nc
        self.cnt = {e: 0 for e in self.ENGS}
        self.known = {e: {} for e in self.ENGS}
        self.prog = {e: [] for e in self.ENGS}
        self.snap = {}
        self.dval = {}
        self.drr = {}
        self.lastw = {}
        self.readers = {}

    def _learn(self, e, ev):
        k = self.known[e]
        if ev[0] == "c":
            sn = self.snap.get((ev[1], ev[2]))
            key, val = ("c", ev[1]), ev[2]
        else:
            sn = ev[3]
            key, val = ("d", ev[1]), ev[2]
        if k.get(key, 0) < val:
            k[key] = val
        if sn:
            for kk, vv in sn.items():
                if k.get(kk, 0) < vv:
                    k[kk] = vv

    def _deps(self, reads, writes):
        evs = []
        for r in reads:
            w = self.lastw.get(r)
            if w is not None:
                evs.append(w)
        for r in writes:
            w = self.lastw.get(r)
            if w is not None:
                evs.append(w)
            evs.extend(self.readers.get(r, ()))
        return evs

    def _emit_waits(self, e, evs):
        waits = {}
        for ev in evs:
            if ev[0] == "c":
                key, val = ("c", ev[1]), ev[2]
            else:
                key, val = ("d", ev[1]), ev[2]
            if self.known[e].get(key, 0) >= val:
                continue
            if waits.get(key, (0, None))[0] < val:
                waits[key] = (val, ev)
        for key, (val, ev) in waits.items():
            if self.known[e].get(key, 0) >= val:
                continue
            self.prog[e].append(("w", key, val))
            self._learn(e, ev)

    def _record(self, ev, reads, writes):
        for r in writes:
            self.lastw[r] = ev
            self.readers[r] = []
        for r in reads:
            if r not in writes:
                self.readers.setdefault(r, []).append(ev)

    def op(self, e, fn, reads=(), writes=()):
        self._emit_waits(e, self._deps(reads, writes))
        self.cnt[e] += 1
        idx = self.cnt[e]
        self.prog[e].append(("o", fn, idx))
        if e == "pe":
            self.known[e][("c", e)] = idx
        self.snap[(e, idx)] = dict(self.known[e])
        ev = ("c", e, idx)
        self._record(ev, reads, writes)
        return ev

    def dma(self, q, fn, reads=(), writes=(), pre="", nsem=None):
        nsem = nsem or self.NDMASEM
        slot = self.drr.get(pre + q, 0) % nsem
        self.drr[pre + q] = self.drr.get(pre + q, 0) + 1
        skey = f"{pre}{q}{slot}"
        prev = self.dval.get(skey, 0)
        evs = self._deps(reads, writes)
        if prev:
            evs.append(("d", skey, prev, None))
        self._emit_waits(q, evs)
        val = prev + 16
        self.dval[skey] = val
        self.prog[q].append(("d", fn, skey))
        ev = ("d", skey, val, dict(self.known[q]))
        self._record(ev, reads, writes)
        return ev

    def finish(self):
        evs = [("c", f, self.cnt[f]) for f in ("pe", "act", "dve", "pool") if self.cnt[f]]
        for skey, v in self.dval.items():
            evs.append(("d", skey, v, None))
        self._emit_waits("sp", evs)

    def emit(self):
        import contextlib
        nc = self.nc
        self.finish()
        with contextlib.ExitStack() as st:
            semh = {}
            for e in ("pe", "act", "dve", "pool"):
                semh[("c", e)] = st.enter_context(nc.semaphore(f"c_{e}"))
            for skey in self.dval:
                semh[("d", skey)] = st.enter_context(nc.semaphore(f"d_{skey}"))
            st.enter_context(nc.allow_non_contiguous_dma(reason="small strided pieces are intentional"))
            block = st.enter_context(nc.Block())

            def run(e):
                def body(engine):
                    for it in self.prog[e]:
                        if it[0] == "w":
                            engine.wait_ge(semh[it[1]], it[2])
                        elif it[0] == "o":
                            it[1](engine).then_inc(semh[("c", e)], 1)
                        else:
                            it[1](engine).then_inc(semh[("d", it[2])], 16)
                return body
            block.tensor(run("pe"))
            block.scalar(run("act"))
            block.vector(run("dve"))
            block.gpsimd(run("pool"))
            block.sync(run("sp"))


class Ctx:
    def __init__(self):
        self.nc = bass.Bass("TRN2", target_bir_lowering=False)
        self.S = Sched(self.nc)
        self.rr = 0

    def din(self, name, shape, dt=F32):
        return self.nc.dram_tensor(name, list(shape), dt, kind="ExternalInput").ap()

    def dout(self, name, shape, dt=F32):
        return self.nc.dram_tensor(name, list(shape), dt, kind="ExternalOutput").ap()

    def dscr(self, name, shape, dt=F32):
        return self.nc.dram_tensor(name, list(shape), dt).ap()

    def sb(self, name, shape, dt=F32):
        return self.nc.alloc_sbuf_tensor("sb_" + name, list(shape), dt)

    def ps(self, name, shape, dt=F32):
        return self.nc.alloc_psum_tensor("ps_" + name, list(shape), dt)

    def load(self, out, in_, w, r=(), q="sp"):
        return self.S.dma(q, lambda e: e.dma_start(out=out, in_=in_), reads=list(r), writes=list(w))

    def store(self, out, in_, r, w=(), q="pool"):
        return self.S.dma(q, lambda e: e.dma_start(out=out, in_=in_), reads=list(r), writes=list(w))

    def castdma(self, out, in_, w, r=()):
        return self.S.dma("pool", lambda e: e.dma_start(out=out, in_=in_), reads=list(r), writes=list(w), pre="cv", nsem=40)

    def mm(self, out, lhsT, rhs, start, stop, r, w):
        return self.S.op("pe", lambda e: e.matmul(out, lhsT=lhsT, rhs=rhs, start=start, stop=stop),
                         reads=list(r), writes=list(w))

    def act(self, out, in_, func, r, w, bias=None, scale=None):
        kw = {}
        if bias is not None:
            kw["bias"] = bias
        if scale is not None:
            kw["scale"] = scale
        return self.S.op("act", lambda e: e.activation(out=out, in_=in_, func=func, **kw),
                         reads=list(r), writes=list(w))

    def tt(self, eng, out, in0, in1, op, r, w):
        return self.S.op(eng, lambda e: e.tensor_tensor(out=out, in0=in0, in1=in1, op=op),
                         reads=list(r), writes=list(w))

    def ts(self, eng, out, in0, s1, op0, r, w, s2=None, op1=None):
        if op1 is None:
            return self.S.op(eng, lambda e: e.tensor_scalar(out=out, in0=in0, scalar1=s1, scalar2=None, op0=op0),
                             reads=list(r), writes=list(w))
        return self.S.op(eng, lambda e: e.tensor_scalar(out=out, in0=in0, scalar1=s1, scalar2=s2, op0=op0, op1=op1),
                         reads=list(r), writes=list(w))

    def stt(self, eng, out, in0, scalar, in1, op0, op1, r, w):
        return self.S.op(eng, lambda e: e.scalar_tensor_tensor(out=out, in0=in0, scalar=scalar, in1=in1, op0=op0, op1=op1),
                         reads=list(r), writes=list(w))

    def copy(self, eng, out, in_, r, w):
        if eng == "act":
            return self.act(out, in_, AF.Copy, r, w)
        return self.S.op(eng, lambda e: e.tensor_copy(out=out, in_=in_), reads=list(r), writes=list(w))

    def anyeng(self, choices=("dve", "pool", "act")):
        self.rr += 1
        return choices[self.rr % len(choices)]


def fm(ap):
    return ap.rearrange("(c p) t -> p c t", p=128)


NA = 24576 // NCORES


def build_L0():
    C = Ctx()
    S = C.S
    cond = C.din("cond", [128, 32, 2])
    wada = C.din("wada", [2, D, NA])
    bada = C.din("bada", [2, 2, NA])
    mod = C.dout("mod", [2, 2, NA])
    sc = C.sb("sc", [128, 32, 2])
    bt = C.sb("bt", [2, 2, NA])
    res = C.sb("res", [2, 2, NA])
    wt = [C.sb(f"wt{i}", [128, NA]) for i in range(3)]
    pss = [C.ps(f"ps{i}", [2, 512]) for i in range(6)]
    C.load(sc[:], cond, ["sc"])
    C.load(bt[:], bada.rearrange("l i n -> i l n"), ["bt"])
    C.act(sc[:], sc[:], AF.Silu, ["sc"], ["sc"])
    for l in range(2):
        for kc in range(32):
            s = (l * 32 + kc) % 3
            C.load(wt[s][:], wada[l, kc * 128:(kc + 1) * 128, :], [("wt", s)])
            for g in range(6):
                C.mm(pss[g][:], sc[:, kc, :], wt[s][:, g * 512:(g + 1) * 512], kc == 0, kc == 31,
                     ["sc", ("wt", s)], [("ps", g)])
        for g in range(6):
            C.tt("dve", res[:, l, g * 512:(g + 1) * 512], pss[g][:], bt[:, l, g * 512:(g + 1) * 512], ALU.add,
                 [("ps", g), "bt"], ["res"])
    C.store(mod.rearrange("l i n -> i l n"), res[:], ["res"], ["mod"], q="sp")
    S.emit()
    return C.nc


def convert_weights(C, src, dst, rows, cols, xs, wt, piece=3584):
    it = 0
    for kc in range(rows // 128):
        for c0 in range(0, cols, piece):
            w = min(piece, cols - c0)
            s = it % 2
            it += 1
            xf = xs[s][:].rearrange("p c t -> p (c t)")[:, 0:w]
            wf = wt[s][:].rearrange("p c t -> p (c t)")[:, 0:w]
            C.load(xf, src[kc * 128:(kc + 1) * 128, c0:c0 + w], [("xs", s)])
            C.copy(C.anyeng(), wf, xf, [("xs", s)], [("wt", s)])
            C.store(dst[kc * 128:(kc + 1) * 128, c0:c0 + w], wf, [("wt", s)], ["wbf"])


def rms_modulate(C, src_fm, t0, T, xs, hT, sq, ssq, rstd, tmp, ones, gs, shift, h32=None):
    for cgi in range(4):
        s = cgi % 2
        C.load(xs[s][:, :, 0:T], src_fm[:, cgi * 8:(cgi + 1) * 8, t0:t0 + T], [("xs", s)])
        for c in range(8):
            q = c % 2
            C.act(sq[q][:, 0:T], xs[s][:, c, 0:T], AF.Square, [("xs", s)], [("sq", q)])
            C.mm(ssq[:, 0:T], ones[:], sq[q][:, 0:T], cgi == 0 and c == 0, cgi == 3 and c == 7,
                 [("sq", q), "ones"], ["ssq"])
    C.ts("dve", rstd[:, 0:T], ssq[:, 0:T], 1.0 / D, ALU.mult, ["ssq"], ["rstd"], s2=EPS, op1=ALU.add)
    C.act(rstd[:, 0:T], rstd[:, 0:T], AF.Sqrt, ["rstd"], ["rstd"])
    C.S.op("dve", lambda e, T=T: e.reciprocal(out=rstd[:, 0:T], in_=rstd[:, 0:T]), reads=["rstd"], writes=["rstd"])
    for cgi in range(4):
        s = cgi % 2
        C.load(xs[s][:, :, 0:T], src_fm[:, cgi * 8:(cgi + 1) * 8, t0:t0 + T], [("xs", s)])
        for c in range(8):
            cc = cgi * 8 + c
            q = c % 2
            C.tt("dve", tmp[q][:, 0:T], xs[s][:, c, 0:T], rstd[:, 0:T], ALU.mult, [("xs", s), "rstd"], [("tmp", q)])
            C.act(hT[:, cc, 0:T], tmp[q][:, 0:T], AF.Identity, [("tmp", q), "vec"], ["hT"],
                  bias=shift[:, cc:cc + 1], scale=gs[:, cc:cc + 1])


def build_L1(last, dbg=False):
    C = Ctx()
    S = C.S
    if dbg:
        dbg_h = C.dout("dbg_h", [128, 32, 256], BF16)
        dbg_r = C.dout("dbg_r", [128, 256])
        dbg_w = C.dout("dbg_w", [128, 32, 512], BF16)
    xT = C.din("xT", [D, NEXT])
    xcT = C.din("xcT", [D, LC])
    vecd = C.din("vec", [128, 192])
    w_in = C.din("w_in", [D, INW])
    ropec = C.din("ropec", [128, NEXT])
    ropes = C.din("ropes", [128, NEXT])
    permd = C.din("perm", [128, 128])
    dftd = C.din("dft", [128, 2, 3, 256], BF16)
    qT = C.dout("qT", [2048, TL], BF16)
    kT = C.dout("kT", [512, NEXT], BF16)
    vO = C.dout("v", [NEXT, 512], BF16)
    kcT = C.dout("kcT", [512, LC], BF16)
    vcO = C.dout("vc", [LC, 512], BF16)
    convT = C.dout("convT", [1024, TL], BF16)
    AB = C.dout("AB", [2, TL, 1024], BF16)
    if not last:
        qcT = C.dout("qcT", [2048, LC], BF16)
        convcT = C.dout("convcT", [1024, LC], BF16)
        fourcT = C.dout("fourcT", [1024, LC], BF16)
    wbf = C.dscr("wbf", [D, INW], BF16)
    Z = C.dscr("Z", [1024, NEXT])
    BG = C.dscr("BG", [1024, NEXT], BF16)
    Zc = C.dscr("Zc", [1024, LC + 2])
    BGc = C.dscr("BGc", [1024, LC], BF16)

    xs = [C.sb(f"xs{i}", [128, 8, 512]) for i in range(2)]
    wt = [C.sb(f"wt{i}", [128, 32, 512], BF16) for i in range(2)]
    hT = C.sb("hT", [128, 32, 512], BF16)
    cgs = C.sb("cgs", [128, 8, 512])
    fuT = C.sb("fuT", [128, 8, 512], BF16)
    vec = C.sb("vecs", [128, 192])
    gsL = C.sb("gsL", [128, 32])
    gsC = C.sb("gsC", [128, 32])
    ones = C.sb("ones", [128, 128], BF16)
    perm = C.sb("perm", [128, 128])
    dft = C.sb("dfts", [128, 2, 3, 256], BF16)
    sq = [C.sb(f"sq{i}", [128, 512], BF16) for i in range(2)]
    tmp = [C.sb(f"tmp{i}", [128, 512]) for i in range(2)]
    rstd = C.sb("rstd", [128, 512])
    rc = C.sb("rc", [128, 512])
    rs = C.sb("rs", [128, 512])
    xq = [C.sb(f"xq{i}", [128, 512]) for i in range(2)]
    st16 = [C.sb(f"st16_{i}", [128, 512], BF16) for i in range(3)]
    st32 = [C.sb(f"st32_{i}", [128, 514]) for i in range(2)]
    zero = C.sb("zero", [128, 2])
    abc = C.sb("abc", [128, 2, 2, 1024], BF16)
    ssq = C.ps("ssq", [128, 512])
    pacc = [C.ps(f"pacc{i}", [128, 512]) for i in range(3)]
    pperm = [C.ps(f"pperm{i}", [128, 512]) for i in range(2)]

    C.load(vec[:], vecd, ["vec"])
    C.load(perm[:], permd, ["perm"])
    C.load(dft[:], dftd, ["dft"])
    S.op("pool", lambda e: e.memset(ones[:], 1.0), writes=["ones"])
    S.op("pool", lambda e: e.memset(zero[:], 0.0), writes=["zero"])
    C.ts("dve", gsL[:], vec[:, 64:96], 1.0, ALU.add, ["vec"], ["vec"])
    C.tt("dve", gsL[:], gsL[:], vec[:, 0:32], ALU.mult, ["vec"], ["vec"])
    C.ts("dve", gsC[:], vec[:, 128:160], 1.0, ALU.add, ["vec"], ["vec"])
    C.tt("dve", gsC[:], gsC[:], vec[:, 0:32], ALU.mult, ["vec"], ["vec"])
    for k8 in range(16):
        C.castdma(wbf[k8 * 256:(k8 + 1) * 256, :], w_in[k8 * 256:(k8 + 1) * 256, :], ["wbf"])
    wbf_fm = fm(wbf)
    C.store(Zc[:, 0:1].rearrange("(c p) t -> p c t", p=128), zero[:, 0:1].unsqueeze(1).to_broadcast([128, 8, 1]), ["zero"], ["Zc"])
    C.store(Zc[:, LC + 1:LC + 2].rearrange("(c p) t -> p c t", p=128), zero[:, 0:1].unsqueeze(1).to_broadcast([128, 8, 1]), ["zero"], ["Zc"])

    tiles = [("HL", 0, 128, "halo"), ("L0", 128, 512, "lat"), ("L1", 640, 512, "lat"),
             ("L2", 1152, 512, "lat"), ("L3", 1664, 512, "lat"), ("HR", 2176, 128, "halo"), ("C", 0, LC, "ctx")]
    wl = 0
    pa = 0
    s16 = 0
    for name, t0, T, kind in tiles:
        isctx = kind == "ctx"
        src = fm(xcT) if isctx else fm(xT)
        rms_modulate(C, src, t0, T, xs, hT, sq, ssq, rstd, tmp, ones,
                     gsC if isctx else gsL, vec[:, 96:128] if isctx else vec[:, 32:64])
        if dbg and isctx:
            C.store(dbg_h, hT[:, :, 0:256], ["hT"], ["dbgh"])
            C.store(dbg_r, rstd[:, 0:256], ["rstd"], ["dbgr"])
        if kind == "lat" or kind == "halo":
            C.load(rc[:, 0:T], ropec[:, t0:t0 + T], ["rc"])
            C.load(rs[:, 0:T], ropes[:, t0:t0 + T], ["rs"])
        if kind == "lat" or (isctx and not last):
            groups = list(range(14))
        elif kind == "halo":
            groups = [4, 5, 8, 9, 10, 11]
        else:
            groups = [4, 5]
        for g in groups:
            ws = wl % 2
            wl += 1
            C.load(wt[ws][:], wbf_fm[:, :, g * 512:(g + 1) * 512], [("wt", ws)], ["wbf"])
            if dbg and isctx and g == 5:
                C.store(dbg_w, wt[ws][:], [("wt", ws)], ["dbgw"])
            if g == 5:
                for tb in range(T // 128):
                    p = pacc[pa % 3]; pk = ("pacc", pa % 3); pa += 1
                    for kc in range(32):
                        C.mm(p[:], hT[:, kc, tb * 128:(tb + 1) * 128], wt[ws][:, kc, :], kc == 0, kc == 31,
                             ["hT", ("wt", ws)], [pk])
                    sb_ = st16[s16 % 3]; sk = ("st16", s16 % 3); s16 += 1
                    C.copy("act", sb_[:], p[:], [pk], [sk])
                    dst = vcO[tb * 128:(tb + 1) * 128, :] if isctx else vO[t0 + tb * 128:t0 + (tb + 1) * 128, :]
                    C.store(dst, sb_[:], [sk], ["vout"])
                continue
            for j in range(4):
                n = 4 * g + j
                p = pacc[pa % 3]; pk = ("pacc", pa % 3); pa += 1
                for kc in range(32):
                    C.mm(p[:, 0:T], wt[ws][:, kc, j * 128:(j + 1) * 128], hT[:, kc, 0:T], kc == 0, kc == 31,
                         ["hT", ("wt", ws)], [pk])
                if g <= 4:
                    sb_ = st16[s16 % 3]; sk = ("st16", s16 % 3); s16 += 1
                    if isctx:
                        C.copy("act", sb_[:, 0:T], p[:, 0:T], [pk], [sk])
                        dst = qcT[n * 128:(n + 1) * 128, :] if g < 4 else kcT[(n - 16) * 128:(n - 15) * 128, :]
                    else:
                        xi = pa % 2
                        C.copy("act", xq[xi][:, 0:T], p[:, 0:T], [pk], [("xq", xi)])
                        C.mm(pperm[xi][:, 0:T], perm[:], xq[xi][:, 0:T], True, True, ["perm", ("xq", xi)], [("pperm", xi)])
                        C.tt("dve", tmp[xi][:, 0:T], pperm[xi][:, 0:T], rs[:, 0:T], ALU.mult, [("pperm", xi), "rs"], [("tmp", xi)])
                        C.tt("pool", xq[xi][:, 0:T], xq[xi][:, 0:T], rc[:, 0:T], ALU.mult, [("xq", xi), "rc"], [("xq", xi)])
                        C.tt("dve", sb_[:, 0:T], xq[xi][:, 0:T], tmp[xi][:, 0:T], ALU.add, [("xq", xi), ("tmp", xi)], [sk])
                        if g < 4:
                            dst = qT[n * 128:(n + 1) * 128, t0 - 128:t0 - 128 + T]
                        else:
                            dst = kT[(n - 16) * 128:(n - 15) * 128, t0:t0 + T]
                    C.store(dst, sb_[:, 0:T], [sk], ["qkout"])
                elif g in (6, 7):
                    sb_ = st16[s16 % 3]; sk = ("st16", s16 % 3); s16 += 1
                    C.copy("act", sb_[:, 0:T], p[:, 0:T], [pk], [sk])
                    c8 = n - 24
                    dst = BGc[c8 * 128:(c8 + 1) * 128, :] if isctx else BG[c8 * 128:(c8 + 1) * 128, t0:t0 + T]
                    C.store(dst, sb_[:, 0:T], [sk], ["BG"])
                elif g in (8, 9):
                    C.copy("act", cgs[:, n - 32, 0:T], p[:, 0:T], [pk], ["cgs"])
                elif g in (10, 11):
                    c8 = n - 40
                    zi = pa % 2
                    zt = st32[zi]; zk = ("st32", zi)
                    if kind == "halo":
                        fl = vec[:, 184:185] if name == "HL" else vec[:, 185:186]
                        C.stt("dve", zt[:, 0:T], cgs[:, c8, 0:T], fl, p[:, 0:T], ALU.mult, ALU.mult, ["cgs", pk, "vec"], [zk])
                    else:
                        C.tt("dve", zt[:, 0:T], cgs[:, c8, 0:T], p[:, 0:T], ALU.mult, ["cgs", pk], [zk])
                    dst = Zc[c8 * 128:(c8 + 1) * 128, 1:1 + LC] if isctx else Z[c8 * 128:(c8 + 1) * 128, t0:t0 + T]
                    C.store(dst, zt[:, 0:T], [zk], ["Z"])
                else:
                    C.copy("act", fuT[:, n - 48, 0:T], p[:, 0:T], [pk], ["fuT"])
        if kind == "lat" or (isctx and not last):
            for tb in range(T // 128):
                for gi in range(4):
                    p = pacc[pa % 3]; pk = ("pacc", pa % 3); pa += 1
                    for cs in range(2):
                        for kc in range(2):
                            C.mm(p[:, cs * 256:(cs + 1) * 256], fuT[:, gi * 2 + kc, tb * 128:(tb + 1) * 128], dft[:, kc, cs, :],
                                 cs == 0 and kc == 0, cs == 1 and kc == 1, ["fuT", "dft"], [pk])
                    if isctx:
                        C.copy("act", abc[:, tb, :, gi * 256:(gi + 1) * 256], p[:].rearrange("p (a c) -> p a c", a=2), [pk], ["abc"])
                    else:
                        sb_ = st16[s16 % 3]; sk = ("st16", s16 % 3); s16 += 1
                        C.copy("act", sb_[:], p[:], [pk], [sk])
                        tt0 = t0 - 128 + tb * 128
                        C.store(AB[:, tt0:tt0 + 128, gi * 256:(gi + 1) * 256].rearrange("a t c -> t a c"),
                                sb_[:].rearrange("p (a c) -> p a c", a=2), [sk], ["AB"])
            if isctx:
                for c8 in range(8):
                    p = pacc[pa % 3]; pk = ("pacc", pa % 3); pa += 1
                    i = 0
                    for ab, var in ((0, 0), (1, 2)):
                        for tb in range(2):
                            C.mm(p[:, 0:LC], abc[:, tb, ab, c8 * 128:(c8 + 1) * 128], dft[:, tb, var, :], i == 0, i == 3,
                                 ["abc", "dft"], [pk])
                            i += 1
                    sb_ = st16[s16 % 3]; sk = ("st16", s16 % 3); s16 += 1
                    C.act(sb_[:, 0:LC], p[:, 0:LC], AF.Copy, [pk], [sk], scale=1.0 / 256.0)
                    C.store(fourcT[c8 * 128:(c8 + 1) * 128, :], sb_[:, 0:LC], [sk], ["fourc"])
    jobs = [(Z, BG, convT, 128 + 512 * i, 512, 512 * i) for i in range(4)]
    if not last:
        jobs.append((Zc, BGc, convcT, 1, LC, 0))
    k = 0
    for Zs, BGs, dstT, z0, T, o0 in jobs:
        for c8 in range(8):
            zi = k % 2; k += 1
            zt = st32[zi]; zk = ("st32", zi)
            C.load(zt[:, 0:T + 2], Zs[c8 * 128:(c8 + 1) * 128, z0 - 1:z0 + T + 1], [zk], ["Z"])
            bgt = st16[s16 % 3]; bk = ("st16", s16 % 3); s16 += 1
            bo = z0 if Zs is Z else 0
            C.load(bgt[:, 0:T], BGs[c8 * 128:(c8 + 1) * 128, bo:bo + T], [bk], ["BG"])
            y = tmp[zi]; yk = ("tmp", zi)
            C.ts("dve", y[:, 0:T], zt[:, 0:T], vec[:, 160 + c8:161 + c8], ALU.mult, [zk, "vec"], [yk])
            C.stt("dve", y[:, 0:T], zt[:, 1:T + 1], vec[:, 168 + c8:169 + c8], y[:, 0:T], ALU.mult, ALU.add, [zk, "vec", yk], [yk])
            C.stt("dve", y[:, 0:T], zt[:, 2:T + 2], vec[:, 176 + c8:177 + c8], y[:, 0:T], ALU.mult, ALU.add, [zk, "vec", yk], [yk])
            ob = st16[s16 % 3]; ok = ("st16", s16 % 3); s16 += 1
            C.tt("pool", ob[:, 0:T], y[:, 0:T], bgt[:, 0:T], ALU.mult, [yk, bk], [ok])
            C.store(dstT[c8 * 128:(c8 + 1) * 128, o0:o0 + T], ob[:, 0:T], [ok], ["convout"])
    S.emit()
    return C.nc


def fmvec(v):
    v = np.asarray(v, np.float32)
    return np.ascontiguousarray(v.reshape(-1, 128).T)


def rope_tables():
    inv = 10000.0 ** (-np.arange(0, 64, 2, dtype=np.float64) / 64.0)
    t = np.arange(SEQ)
    row, col = (t // 64).astype(np.float64), (t % 64).astype(np.float64)
    d = np.arange(128)
    axis, within = d // 64, d % 64
    ph, f = within // 32, within % 32
    pos = np.where(axis[:, None] == 0, row[None, :], col[None, :])
    ang = (pos * inv[f][:, None]).astype(np.float32).astype(np.float64)
    cos = np.cos(ang).astype(np.float32)
    sin = (np.sin(ang) * np.where(ph == 0, -1.0, 1.0)[:, None]).astype(np.float32)
    perm = np.zeros((128, 128), np.float32)
    partner = np.where(ph == 0, d + 32, d - 32)
    perm[partner, d] = 1.0
    return cos, sin, perm


def dft256_table():
    k = np.arange(256, dtype=np.float64)
    ang = 2 * np.pi * np.outer(k, k) / 256.0
    t = np.stack([np.cos(ang), np.sin(ang), -np.sin(ang)], axis=1)
    return np.ascontiguousarray(t.reshape(2, 128, 3, 256).transpose(1, 0, 2, 3)).astype(NPBF)


def ext_slices(x2d):
    xp = np.zeros((SEQ + 256, x2d.shape[1]), x2d.dtype)
    xp[128:128 + SEQ] = x2d
    return [np.ascontiguousarray(xp[j * TL:j * TL + NEXT].T) for j in range(NCORES)]


def make_vec(j, g, shiftL, scaleL, shiftC, scaleC, conv_w):
    v = np.zeros((128, 192), np.float32)
    v[:, 0:32] = fmvec(g)
    v[:, 32:64] = fmvec(shiftL)
    v[:, 64:96] = fmvec(scaleL)
    v[:, 96:128] = fmvec(shiftC)
    v[:, 128:160] = fmvec(scaleC)
    for jj in range(3):
        v[:, 160 + jj * 8:168 + jj * 8] = fmvec(conv_w[jj])
    v[:, 184] = 0.0 if j == 0 else 1.0
    v[:, 185] = 0.0 if j == NCORES - 1 else 1.0
    return v


def build_L2():
    C = Ctx()
    S = C.S
    Ain = C.din("Ain", [SEQ, 128], BF16)
    Bin = C.din("Bin", [SEQ, 128], BF16)
    Wd = C.din("W", [128, 4, 128], BF16)
    twd = C.din("tw", [128, 2, 128])
    yT = C.dout("yT", [128, SEQ], BF16)
    ZS = C.dscr("ZS", [2, 128, 128, 128], BF16)
    XA = C.sb("XA", [128, SEQ], BF16)
    XB = C.sb("XB", [128, SEQ], BF16)
    ZT = [C.sb(f"ZT{i}", [128, 128, 128], BF16) for i in range(2)]
    W = C.sb("W", [128, 4, 128], BF16)
    tw = C.sb("tw", [128, 2, 128])
    ta = [C.sb(f"ta{i}", [128, 512]) for i in range(2)]
    tb_ = [C.sb(f"tb{i}", [128, 512]) for i in range(2)]
    zo = [[C.sb(f"zo{i}{x}", [128, 512], BF16) for x in range(2)] for i in range(2)]
    pz = [[C.ps(f"pz{i}{x}", [128, 512]) for x in range(2)] for i in range(2)]
    pc = [C.ps(f"pc{i}", [128, 512]) for i in range(2)]
    C.load(W[:], Wd, ["W"])
    C.load(tw[:], twd, ["tw"])
    for h in range(4):
        sl = slice(h * 4096, (h + 1) * 4096)
        C.load(XA[:, sl], Ain.rearrange("(a b) c -> a (b c)", a=128)[:, sl], [("XA", h)])
        C.load(XB[:, sl], Bin.rearrange("(a b) c -> a (b c)", a=128)[:, sl], [("XB", h)], q="pool")
    for g in range(32):
        i = g % 2
        cs = slice(g * 512, (g + 1) * 512)
        h = g // 8
        C.mm(pz[i][0][:], W[:, 0, :], XA[:, cs], True, False, ["W", ("XA", h)], [("pz", i, 0)])
        C.mm(pz[i][0][:], W[:, 2, :], XB[:, cs], False, True, ["W", ("XB", h)], [("pz", i, 0)])
        C.mm(pz[i][1][:], W[:, 2, :], XA[:, cs], True, False, ["W", ("XA", h)], [("pz", i, 1)])
        C.mm(pz[i][1][:], W[:, 3, :], XB[:, cs], False, True, ["W", ("XB", h)], [("pz", i, 1)])
        for k in range(4):
            t2 = g * 4 + k
            ks = slice(k * 128, (k + 1) * 128)
            C.act(ta[i][:, ks], pz[i][0][:, ks], AF.Copy, [("pz", i, 0), "tw"], [("ta", i)], scale=tw[:, 0, t2:t2 + 1])
            C.act(tb_[i][:, ks], pz[i][0][:, ks], AF.Copy, [("pz", i, 0), "tw"], [("tb", i)], scale=tw[:, 1, t2:t2 + 1])
        for k in range(4):
            t2 = g * 4 + k
            ks = slice(k * 128, (k + 1) * 128)
            C.stt("dve", zo[i][0][:, ks], pz[i][1][:, ks], tw[:, 1, t2:t2 + 1], ta[i][:, ks], ALU.mult, ALU.add,
                  [("pz", i, 1), "tw", ("ta", i)], [("zo", i, 0)])
            C.stt("dve", zo[i][1][:, ks], pz[i][1][:, ks], tw[:, 0, t2:t2 + 1], tb_[i][:, ks], ALU.mult, ALU.subtract,
                  [("pz", i, 1), "tw", ("tb", i)], [("zo", i, 1)])
        for x in range(2):
            C.store(ZS[x, :, g * 4:(g + 1) * 4, :], zo[i][x][:].rearrange("p (a c) -> p a c", a=4), [("zo", i, x)], ["ZS"],
                    q="sp" if x == 0 else "pool")
    for x in range(2):
        for h in range(4):
            C.load(ZT[x][:, h * 32:(h + 1) * 32, :], ZS[x, h * 32:(h + 1) * 32, :, :].rearrange("f t c -> t f c"),
                   [("ZT", x, h)], ["ZS"], q="sp" if x == 0 else "pool")
    Y = XA
    Y3 = Y[:].rearrange("p (f2 f1) -> p f2 f1", f1=128)
    for fg in range(32):
        i = fg % 2
        for k in range(4):
            f1 = fg * 4 + k
            ks = slice(k * 128, (k + 1) * 128)
            C.mm(pc[i][:, ks], ZT[0][:, f1, :], W[:, 0, :], k == 0, False, ["W", ("ZT", 0, f1 // 32)], [("pc", i)])
            C.mm(pc[i][:, ks], ZT[1][:, f1, :], W[:, 1, :], False, k == 3, ["W", ("ZT", 1, f1 // 32)], [("pc", i)])
        C.act(Y3[:, :, fg * 4:(fg + 1) * 4], pc[i][:].rearrange("p (a f) -> p f a", a=4), AF.Copy, [("pc", i)],
              [("XA", 0), ("XA", 1), ("XA", 2), ("XA", 3)], scale=1.0 / 2048.0)
    for h in range(4):
        sl = slice(h * 4096, (h + 1) * 4096)
        C.store(yT[:, sl], Y[:, sl], [("XA", h)], ["yT"], q="sp" if h % 2 == 0 else "pool")
    S.emit()
    return C.nc


def fft_tables():
    k = np.arange(128, dtype=np.float64)
    ang = 2 * np.pi * np.outer(k, k) / 128.0
    W = np.stack([np.cos(ang), np.sin(ang), -np.sin(ang), -np.cos(ang)], axis=1).astype(NPBF)
    ang2 = 2 * np.pi * np.outer(k, k) / float(SEQ)
    tw = np.stack([np.cos(ang2), np.sin(ang2)], axis=1).astype(np.float32)
    return np.ascontiguousarray(W), np.ascontiguousarray(tw)


def build_L3(last, NEe=NE, dbg=False):
    C = Ctx()
    S = C.S
    if dbg:
        dbg_mix = C.dout("dbg_mix", [128, 32, 512], BF16)
        dbg_xm = C.dout("dbg_xm", [128, 32, 512])
        dbg_wt = C.dout("dbg_wt", [128, 8192], BF16)
    qT = C.din("qT", [2048, TL], BF16)
    kT = C.din("kT", [512, NEXT], BF16)
    vI = C.din("v", [NEXT, 512], BF16)
    kcT = C.din("kcT", [512, LC], BF16)
    vcI = C.din("vc", [LC, 512], BF16)
    convT = C.din("convT", [1024, TL], BF16)
    fourT = C.din("fourT", [1024, TL], BF16)
    xT = C.din("xT", [D, TL])
    if not last:
        qcT = C.din("qcT", [2048, LC], BF16)
        convcT = C.din("convcT", [1024, LC], BF16)
        fourcT = C.din("fourcT", [1024, LC], BF16)
        xcT = C.din("xcT", [D, LC])
        xocT = C.dout("xocT", [D, LC])
    vecd = C.din("vec2", [128, 336])
    maskd = C.din("masks", [128, 4, 512], BF16)
    w_out = C.din("w_out", [D, D])
    wrd = C.din("wr", [128, 32, 32])
    brd = C.din("br", [128, 32])
    w_gu = C.din("w_gu", [NEe, 6, 128, 8192])
    bgud = C.din("bgu", [128, NEe, 12])
    w_dn = C.din("w_dn", [NEe, 128, 24576])
    bdnd = C.din("bdnT", [128, 32, NEe])
    identd = C.din("ident", [128, 128])
    xoT = C.dout("xoT", [D, TL])
    wobf = C.dscr("wobf", [D, D], BF16)
    wgubf = [C.dscr(f"wgubf{i}", [6, 128, 8192], BF16) for i in range(NEe)]
    wdnbf = [C.dscr(f"wdnbf{i}", [128, 24576], BF16) for i in range(NEe)]
    XM = C.dscr("XM", [D, 512])
    GT = C.dscr("GT", [32, 512])

    xt = C.sb("xt", [128, 32, 512])
    mix = C.sb("mix", [128, 32, 512], BF16)
    wt = [C.sb(f"wt{i}", [128, 32, 256], BF16) for i in range(2)]
    actT = C.sb("actT", [128, 6, 512], BF16)
    vec = C.sb("vec", [128, 336])
    gs2L = C.sb("gs2L", [128, 32])
    gs2C = C.sb("gs2C", [128, 32])
    masks = C.sb("masks", [128, 4, 512], BF16)
    wr = C.sb("wr", [128, 32, 32])
    br = C.sb("br", [128, 32])
    bgu = C.sb("bgu", [128, NEe, 12])
    bdn = C.sb("bdnT", [128, 32, NEe])
    ident = C.sb("ident", [128, 128])
    ones = C.sb("ones", [128, 128], BF16)
    zeros = C.sb("zeros", [128, 128])
    sinkrow = C.sb("sinkrow", [128, 16, 128])
    Kc = C.sb("Kc", [128, 4, LC], BF16)
    Vc = C.sb("Vc", [128, 2, 512], BF16)
    Kt = [C.sb(f"Kt{i}", [128, 768], BF16) for i in range(2)]
    Vt = [C.sb(f"Vt{i}", [128, 6, 128], BF16) for i in range(2)]
    Qt0 = C.sb("Qt0", [128, 4, 512], BF16)
    Qt = [Qt0, Qt0]
    Ej = [C.sb(f"E{i}", [128, 512], BF16) for i in range(4)]
    sq = [C.sb(f"sq{i}", [128, 512], BF16) for i in range(2)]
    tmp = [C.sb(f"tmp{i}", [128, 512]) for i in range(4)]
    rstd = C.sb("rstd", [128, 512])
    den = C.sb("den", [128, 512])
    gbs2 = [C.sb(f"gbs{i}", [128, 512]) for i in range(2)]
    gT = C.sb("gT", [32, 512])
    rt = C.sb("rt", [128, 96])
    rt2 = C.sb("rt2", [128, 16])
    bank = [C.ps(f"bank{i}", [128, 512]) for i in range(8)]
    bk = lambda i: ("bank", i)

    C.load(vec[:], vecd, ["vec"])
    C.load(masks[:], maskd, ["masks"])
    C.load(wr[:], wrd, ["wr"])
    C.load(br[:], brd, ["br"])
    C.load(bgu[:], bgud, ["bgu"])
    C.load(bdn[:], bdnd, ["bdn"])
    C.load(ident[:], identd, ["ident"])
    C.load(Kc[:], kcT.rearrange("(h p) t -> p h t", p=128), ["Kc"])
    C.load(Vc[:], vcI.rearrange("(b p) d -> p b d", p=128), ["Vc"])
    S.op("pool", lambda e: e.memset(ones[:], 1.0), writes=["ones"])
    S.op("pool", lambda e: e.memset(zeros[:], 0.0), writes=["zeros"])
    for h in range(16):
        C.act(sinkrow[:, h, :], zeros[:], AF.Exp, ["zeros", "vec"], ["sinkrow"], bias=vec[:, 320 + h:321 + h])
    C.ts("dve", gs2L[:], vec[:, 96:128], 1.0, ALU.add, ["vec"], ["gs2"])
    C.tt("dve", gs2L[:], gs2L[:], vec[:, 32:64], ALU.mult, ["vec", "gs2"], ["gs2"])
    C.ts("dve", gs2C[:], vec[:, 224:256], 1.0, ALU.add, ["vec"], ["gs2"])
    C.tt("dve", gs2C[:], gs2C[:], vec[:, 32:64], ALU.mult, ["vec", "gs2"], ["gs2"])

    for k8 in range(8):
        C.castdma(wobf[k8 * 512:(k8 + 1) * 512, :], w_out[k8 * 512:(k8 + 1) * 512, :], ["wbfo"])
    for e_ in range(NEe):
        for fc in range(6):
            C.castdma(wgubf[e_][fc], w_gu[e_, fc], [("wbfe", e_)])
        for q4 in range(6):
            C.castdma(wdnbf[e_][:, q4 * 4096:(q4 + 1) * 4096], w_dn[e_, :, q4 * 4096:(q4 + 1) * 4096], [("wbfe", e_)])
    allxt = [("xt", c) for c in range(32)]

    wobf_fm = fm(wobf)
    tiles = [(f"L{i}", 512 * i, 512, "lat") for i in range(4)]
    if not last:
        tiles.append(("C", 0, LC, "ctx"))
    wl = 0
    pa = 0
    ei = 0
    qi = 0
    ti = 0
    wdl = 0
    for name, t0, T, kind in tiles:
        isctx = kind == "ctx"
        pe2 = "dve" if name == "L0" else "pool"
        xsrc = fm(xcT) if isctx else fm(xT)
        xdst = fm(xocT) if isctx else fm(xoT)
        o_ = 160 if isctx else 0
        gmix = vec[:, 160:192] if isctx else vec[:, 0:32]
        shift2 = vec[:, 192:224] if isctx else vec[:, 64:96]
        gffn = vec[:, 256:288] if isctx else vec[:, 128:160]
        gs2 = gs2C if isctx else gs2L
        for cgi in range(4):
            C.load(xt[:, cgi * 8:(cgi + 1) * 8, 0:T], xsrc[:, cgi * 8:(cgi + 1) * 8, t0:t0 + T],
                   [("xt", c) for c in range(cgi * 8, cgi * 8 + 8)])
        for kvh in range(4):
            ks_ = (kvh + (0 if isctx else 0)) % 2
            if not isctx:
                C.load(Kt[ks_][:], kT[kvh * 128:(kvh + 1) * 128, t0:t0 + 768], [("Kt", ks_)])
                C.load(Vt[ks_][:], vI[t0:t0 + 768, kvh * 128:(kvh + 1) * 128].rearrange("(b p) d -> p b d", p=128), [("Vt", ks_)])
            qsrc = qcT if isctx else qT
            C.load(Qt[ks_][:, :, 0:T], qsrc[kvh * 512:(kvh + 1) * 512, t0:t0 + T].rearrange("(g p) t -> p g t", p=128), [("Qt", 0)])
            for b in range(T // 128):
                blocks = [("ctx", 0), ("ctx", 1)] if isctx else [("loc", 0), ("loc", 1), ("loc", 2), ("ctx", 0), ("ctx", 1)]
                pO = 3 + (qi % 2)
                pD = 5 + (qi % 2)
                qi += 1
                for bi, (bt, jb) in enumerate(blocks):
                    pS = pa % 3
                    pa += 1
                    if bt == "loc":
                        lk = Kt[ks_][:, (b + jb) * 128:(b + jb + 1) * 128]
                        lv = Vt[ks_][:, b + jb, :]
                        rk = [("Kt", ks_)]
                        rv = [("Vt", ks_)]
                    else:
                        lk = Kc[:, kvh, jb * 128:(jb + 1) * 128]
                        lv = Vc[:, jb, kvh * 128:(kvh + 1) * 128]
                        rk = ["Kc"]
                        rv = ["Vc"]
                    C.mm(bank[pS][:].rearrange("p (g q) -> p g q", g=4), lk, Qt[ks_][:, :, b * 128:(b + 1) * 128], True, True,
                         rk + [("Qt", 0)], [bk(pS)])
                    E = Ej[ei % 4]
                    ek = ("E", ei % 4)
                    ei += 1
                    C.act(E[:], bank[pS][:], AF.Exp, [bk(pS)], [ek], scale=float(128 ** -0.5))
                    if bt == "loc" and jb != 1:
                        if jb == 0:
                            mi = 0 if (name == "L0" and b == 0) else 1
                        else:
                            mi = 3 if (name == "L3" and b == 3) else 2
                        C.tt("dve", E[:], E[:], masks[:, mi, :], ALU.mult, [ek, "masks"], [ek])
                    C.mm(bank[pO][:], lv, E[:], bi == 0, bi == len(blocks) - 1, rv + [ek], [bk(pO)])
                    C.mm(bank[pD][:], ones[:], E[:], bi == 0, bi == len(blocks) - 1, ["ones", ek], [bk(pD)])
                C.tt("dve", den[:], bank[pD][:], sinkrow[:, kvh * 4:(kvh + 1) * 4, :].rearrange("p g q -> p (g q)"), ALU.add,
                     [bk(pD), "sinkrow"], ["den"])
                S.op("dve", lambda e: e.reciprocal(out=den[:], in_=den[:]), reads=["den"], writes=["den"])
                C.tt("dve", mix[:, kvh * 4:(kvh + 1) * 4, b * 128:(b + 1) * 128], bank[pO][:].rearrange("p (g q) -> p g q", g=4),
                     den[:].rearrange("p (g q) -> p g q", g=4), ALU.mult, [bk(pO), "den"], ["mix"])
        csrc, fsrc = (convcT, fourcT) if isctx else (convT, fourT)
        C.load(mix[:, 16:24, 0:T], fm(csrc)[:, :, t0:t0 + T], ["mix"])
        C.load(mix[:, 24:32, 0:T], fm(fsrc)[:, :, t0:t0 + T], ["mix"])
        if dbg and name == "L0":
            C.store(dbg_mix, mix[:], ["mix"], ["dbgmix"])
        for g in range(16):
            ws = wl % 2
            wl += 1
            C.load(wt[ws][:], wobf_fm[:, :, g * 256:(g + 1) * 256], [("wt", ws)], ["wbfo"])
            for j in range(2):
                n = 2 * g + j
                p = pa % 3
                pa += 1
                for kc in range(32):
                    C.mm(bank[p][:, 0:T], wt[ws][:, kc, j * 128:(j + 1) * 128], mix[:, kc, 0:T], kc == 0, kc == 31,
                         ["mix", ("wt", ws)], [bk(p)])
                C.stt("dve", xt[:, n, 0:T], bank[p][:, 0:T], gmix[:, n:n + 1], xt[:, n, 0:T], ALU.mult, ALU.add,
                      [bk(p), "vec", ("xt", n)], [("xt", n)])
        if dbg and name == "L0":
            C.store(dbg_xm, xt[:], allxt, ["dbgxm"])
        for c in range(32):
            q = c % 2
            C.act(sq[q][:, 0:T], xt[:, c, 0:T], AF.Square, [("xt", c)], [("sq", q)])
            C.mm(bank[7][:, 0:T], ones[:], sq[q][:, 0:T], c == 0, c == 31, [("sq", q), "ones"], [bk(7)])
        C.ts("dve", rstd[:, 0:T], bank[7][:, 0:T], 1.0 / D, ALU.mult, [bk(7)], ["rstd"], s2=EPS, op1=ALU.add)
        C.act(rstd[:, 0:T], rstd[:, 0:T], AF.Sqrt, ["rstd"], ["rstd"])
        S.op("dve", lambda e, T=T: e.reciprocal(out=rstd[:, 0:T], in_=rstd[:, 0:T]), reads=["rstd"], writes=["rstd"])
        nb = T // 128
        for c in range(32):
            q = ti % 2
            q2 = 2 + ti % 2
            ti += 1
            C.tt("dve", tmp[q][:, 0:T], xt[:, c, 0:T], rstd[:, 0:T], ALU.mult, [("xt", c), "rstd"], [("tmp", q)])
            C.act(tmp[q2][:, 0:T], tmp[q][:, 0:T], AF.Identity, [("tmp", q), "vec", "gs2"], [("tmp", q2)],
                  bias=shift2[:, c:c + 1], scale=gs2[:, c:c + 1])
            C.copy(pe2, mix[:, c, 0:T], tmp[q2][:, 0:T], [("tmp", q2)], ["mix"])
            for tb in range(nb):
                C.mm(bank[3 + tb][:, 0:32], tmp[q2][:, tb * 128:(tb + 1) * 128], wr[:, c, :],
                     c == 0, c == 31, [("tmp", q2), "wr"], [bk(3 + tb)])
        for cgi in range(4):
            C.store(fm(XM)[:, cgi * 8:(cgi + 1) * 8, 0:T], xt[:, cgi * 8:(cgi + 1) * 8, 0:T],
                    [("xt", c) for c in range(cgi * 8, cgi * 8 + 8)], [("XM", cgi)], q="sp")
        for tb in range(nb):
            C.tt("dve", rt[:, 0:32], bank[3 + tb][:, 0:32], br[:], ALU.add, [bk(3 + tb), "br"], ["rt"])
            S.op("dve", lambda e: e.max(out=rt2[:, 0:8], in_=rt[:, 0:32]), reads=["rt"], writes=["rt2"])
            C.ts("dve", rt[:, 64:96], rt[:, 0:32], rt2[:, 3:4], ALU.is_ge, ["rt", "rt2"], ["rtm"])
            C.ts("dve", rt2[:, 8:9], rt2[:, 0:1], -1.0, ALU.mult, ["rt2"], ["rt2n"])
            C.act(rt[:, 32:64], rt[:, 0:32], AF.Exp, ["rt", "rt2n"], ["rte"], bias=rt2[:, 8:9])
            C.tt("dve", rt[:, 32:64], rt[:, 32:64], rt[:, 64:96], ALU.mult, ["rte", "rtm"], ["rte"])
            S.op("dve", lambda e: e.reduce_sum(out=rt2[:, 9:10], in_=rt[:, 32:64], axis=mybir.AxisListType.X),
                 reads=["rte"], writes=["rt2s"])
            S.op("dve", lambda e: e.reciprocal(out=rt2[:, 10:11], in_=rt2[:, 9:10]), reads=["rt2s"], writes=["rt2r"])
            C.ts("dve", rt[:, 32:64], rt[:, 32:64], rt2[:, 10:11], ALU.mult, ["rte", "rt2r"], ["rte"])
            gb_ = (0, 1, 2, 7)[tb]
            C.mm(bank[gb_][0:32, 0:128], rt[:, 32:64], ident[:], True, True, ["rte", "ident"], [bk(gb_)])
            C.copy("act", gT[:, tb * 128:(tb + 1) * 128], bank[gb_][0:32, 0:128], [bk(gb_)], ["gT"])
        C.store(GT[:, 0:T], gT[:, 0:T], ["gT"], ["GT"], q="sp")
        S.op(pe2, lambda e, T=T: e.memset(xt[:, :, 0:T], 0.0), reads=[("XM", i) for i in range(4)], writes=allxt)
        for e_ in range(NEe):
            gbs = gbs2[e_ % 2]
            gk = ("gbs", e_ % 2)
            for hh in range(T // 256):
                C.load(gbs[:, hh * 256:(hh + 1) * 256], GT[e_:e_ + 1, hh * 256:(hh + 1) * 256].partition_broadcast(128), [gk], ["GT"])
            for n in range(32):
                C.stt("dve", xt[:, n, 0:T], gbs[:, 0:T], bdn[:, n, e_:e_ + 1], xt[:, n, 0:T], ALU.mult, ALU.add,
                      [gk, "bdn", ("xt", n)], [("xt", n)])
            for fc in range(6):
                ws = wl % 2
                wl += 1
                C.load(wt[ws][:].rearrange("p c t -> p (c t)"), wgubf[e_][fc], [("wt", ws)], [("wbfe", e_)])
                wv = wt[ws][:].rearrange("p c t -> p (c t)").rearrange("p (g k j) -> p g k j", g=2, k=32)
                if dbg and name == "L0" and e_ == 1 and fc == 2:
                    C.store(dbg_wt, wt[ws][:].rearrange("p c t -> p (c t)"), [("wt", ws)], ["dbgwt"])
                pgt = pa % 8
                pa += 1
                put = pa % 8
                pa += 1
                for kc in range(32):
                    C.mm(bank[pgt][:, 0:T], wv[:, 0, kc, :], mix[:, kc, 0:T], kc == 0, kc == 31, ["mix", ("wt", ws)], [bk(pgt)])
                for kc in range(32):
                    C.mm(bank[put][:, 0:T], wv[:, 1, kc, :], mix[:, kc, 0:T], kc == 0, kc == 31, ["mix", ("wt", ws)], [bk(put)])
                g1, sg, u1 = tmp[0], tmp[1], tmp[2]
                C.ts("dve", g1[:, 0:T], bank[pgt][:, 0:T], bgu[:, e_, fc:fc + 1], ALU.add, [bk(pgt), "bgu"], [("tmp", 0)], s2=7.0, op1=ALU.min)
                C.act(sg[:, 0:T], g1[:, 0:T], AF.Sigmoid, [("tmp", 0)], [("tmp", 1)], scale=1.702)
                C.ts("dve", u1[:, 0:T], bank[put][:, 0:T], bgu[:, e_, 6 + fc:7 + fc], ALU.add, [bk(put), "bgu"], [("tmp", 2)], s2=7.0, op1=ALU.min)
                C.ts(pe2, u1[:, 0:T], u1[:, 0:T], -7.0, ALU.max, [("tmp", 2)], [("tmp", 2)], s2=1.0, op1=ALU.add)
                C.tt(pe2, sg[:, 0:T], sg[:, 0:T], g1[:, 0:T], ALU.mult, [("tmp", 0), ("tmp", 1)], [("tmp", 1)])
                C.tt("dve", sg[:, 0:T], sg[:, 0:T], u1[:, 0:T], ALU.mult, [("tmp", 1), ("tmp", 2)], [("tmp", 1)])
                C.tt(pe2, actT[:, fc, 0:T], sg[:, 0:T], gbs[:, 0:T], ALU.mult, [("tmp", 1), gk], [("actT", fc)])
            for ng in range(4):
                ws = wl % 2
                wl += 1
                wflat = wt[ws][:].rearrange("p c t -> p (c t)")[:, 0:6144]
                C.load(wflat, wdnbf[e_][:, ng * 6144:(ng + 1) * 6144], [("wt", ws)], [("wbfe", e_)])
                dv = wflat.rearrange("p (f j) -> p f j", f=6)
                for j in range(8):
                    n = ng * 8 + j
                    p = pa % 8
                    pa += 1
                    for fc in range(6):
                        C.mm(bank[p][:, 0:T], dv[:, fc, j * 128:(j + 1) * 128], actT[:, fc, 0:T], fc == 0, fc == 5,
                             [("wt", ws), ("actT", fc)], [bk(p)])
                    C.tt("dve", xt[:, n, 0:T], xt[:, n, 0:T], bank[p][:, 0:T], ALU.add, [bk(p), ("xt", n)], [("xt", n)])
        for n in range(32):
            q = ti % 4
            ti += 1
            C.load(tmp[q][:, 0:T], fm(XM)[:, n, 0:T], [("tmp", q)], [("XM", n // 8)])
            C.stt("dve", xt[:, n, 0:T], xt[:, n, 0:T], gffn[:, n:n + 1], tmp[q][:, 0:T], ALU.mult, ALU.add,
                  [("xt", n), "vec", ("tmp", q)], [("xt", n)])
        if not last:
            for cgi in range(4):
                C.store(xdst[:, cgi * 8:(cgi + 1) * 8, t0:t0 + T], xt[:, cgi * 8:(cgi + 1) * 8, 0:T],
                        [("xt", c) for c in range(cgi * 8, cgi * 8 + 8)], ["xout"], q="sp")
        else:
            for c in range(32):
                q = c % 2
                C.act(sq[q][:, 0:T], xt[:, c, 0:T], AF.Square, [("xt", c)], [("sq", q)])
                C.mm(bank[7][:, 0:T], ones[:], sq[q][:, 0:T], c == 0, c == 31, [("sq", q), "ones"], [bk(7)])
            C.ts("dve", rstd[:, 0:T], bank[7][:, 0:T], 1.0 / D, ALU.mult, [bk(7)], ["rstd"], s2=EPS, op1=ALU.add)
            C.act(rstd[:, 0:T], rstd[:, 0:T], AF.Sqrt, ["rstd"], ["rstd"])
            S.op("dve", lambda e, T=T: e.reciprocal(out=rstd[:, 0:T], in_=rstd[:, 0:T]), reads=["rstd"], writes=["rstd"])
            for c in range(32):
                q = ti % 4
                ti += 1
                C.tt("dve", tmp[q][:, 0:T], xt[:, c, 0:T], rstd[:, 0:T], ALU.mult, [("xt", c), "rstd"], [("tmp", q)])
                C.act(xt[:, c, 0:T], tmp[q][:, 0:T], AF.Copy, [("tmp", q), "vec"], [("xt", c)], scale=vec[:, 288 + c:289 + c])
            for cgi in range(4):
                C.store(xdst[:, cgi * 8:(cgi + 1) * 8, t0:t0 + T], xt[:, cgi * 8:(cgi + 1) * 8, 0:T],
                        [("xt", c) for c in range(cgi * 8, cgi * 8 + 8)], ["xout"], q="sp")
    S.emit()
    return C.nc


def make_vec2(modL, modC, g_ffn, g_final, sink):
    v = np.zeros((128, 336), np.float32)
    v[:, 0:32] = fmvec(modL[2])
    v[:, 32:64] = fmvec(g_ffn)
    v[:, 64:96] = fmvec(modL[3])
    v[:, 96:128] = fmvec(modL[4])
    v[:, 128:160] = fmvec(modL[5])
    v[:, 160:192] = fmvec(modC[2])
    v[:, 192:224] = fmvec(modC[3])
    v[:, 224:256] = fmvec(modC[4])
    v[:, 256:288] = fmvec(modC[5])
    v[:, 288:320] = fmvec(g_final)
    v[:, 320:336] = np.asarray(sink, np.float32)[None, :]
    return v


def make_masks(j):
    k = np.arange(128)[:, None]
    q = np.arange(128)[None, :]
    mp = np.tile((k >= q).astype(np.float32), (1, 4))
    mn = np.tile((k <= q).astype(np.float32), (1, 4))
    z = np.zeros_like(mp)
    m = np.stack([z if j == 0 else mp, mp, mn, z if j == NCORES - 1 else mn], axis=1)
    return np.ascontiguousarray(m).astype(NPBF)


def expert_layout(w_gu_l, w_dn_l):
    ne = w_gu_l.shape[0]
    a = w_gu_l.reshape(ne, 32, 128, 2, 6, 128).transpose(0, 4, 2, 3, 1, 5)
    a = np.ascontiguousarray(a).reshape(ne, 6, 128, 8192)
    b = w_dn_l.reshape(ne, 6, 128, 4, 1024).transpose(0, 2, 3, 1, 4)
    b = np.ascontiguousarray(b).reshape(ne, 128, 24576)
    return a, b


def moe_small_inputs(w_router, b_router, b_gu, b_dn, ne=NE):
    wr = np.ascontiguousarray(np.asarray(w_router, np.float32).reshape(32, 128, 32).transpose(1, 0, 2))
    br = np.ascontiguousarray(np.broadcast_to(np.asarray(b_router, np.float32)[None, :], (128, 32)))
    bgu = np.ascontiguousarray(np.asarray(b_gu, np.float32)[:ne].reshape(ne, 12, 128).transpose(2, 0, 1))
    bdnT = np.ascontiguousarray(np.asarray(b_dn, np.float32)[:ne].reshape(ne, 32, 128).transpose(2, 1, 0))
    return wr, br, bgu, bdnT


def _run(nc, in_maps):
    return run_bass_kernel_spmd(nc, in_maps, core_ids=list(range(NCORES))).results


def ext_from_fm(xfm_list):
    full = np.concatenate(xfm_list, axis=1)
    pad = np.zeros((full.shape[0], SEQ + 256), full.dtype)
    pad[:, 128:128 + SEQ] = full
    return [np.ascontiguousarray(pad[:, j * TL:j * TL + NEXT]) for j in range(NCORES)]


def kernel(x, c, ctx, c_ctx, w_ada, b_ada, g_mix, w_in, conv_w, attn_sink, w_out, g_ffn,
           w_router, b_router, w_gu, b_gu, w_dn, b_dn, g_final):
    f = lambda a: np.asarray(a, np.float32)
    x, c, ctx, c_ctx = f(x), f(c), f(ctx), f(c_ctx)
    w_ada, b_ada, g_mix, w_in, conv_w, attn_sink = f(w_ada), f(b_ada), f(g_mix), f(w_in), f(conv_w), f(attn_sink)
    w_out, g_ffn, w_router, b_router = f(w_out), f(g_ffn), f(w_router), f(b_router)
    w_gu, b_gu, w_dn, b_dn, g_final = f(w_gu), f(b_gu), f(w_dn), f(b_dn), f(g_final)
    R = range(NCORES)
    cond = np.ascontiguousarray(np.stack([c[0], c_ctx], 0).reshape(2, 32, 128).transpose(2, 1, 0))
    res = _run(build_L0(), [{"cond": cond, "wada": np.ascontiguousarray(w_ada[:, :, j * NA:(j + 1) * NA]),
                             "bada": np.ascontiguousarray(np.broadcast_to(b_ada[:, None, j * NA:(j + 1) * NA], (2, 2, NA)))}
                            for j in R])
    mod = np.concatenate([res[j]["mod"] for j in R], axis=2).reshape(2, 2, 6, D)
    cos, sin, perm = rope_tables()
    cosp = np.zeros((128, SEQ + 256), np.float32)
    cosp[:, 128:128 + SEQ] = cos
    sinp = np.zeros((128, SEQ + 256), np.float32)
    sinp[:, 128:128 + SEQ] = sin
    dft = dft256_table()
    Wf, twf = fft_tables()
    ident = np.eye(128, dtype=np.float32)
    nc2 = build_L2()
    xfm = [np.ascontiguousarray(x[0, j * TL:(j + 1) * TL].T) for j in R]
    xcfm = np.ascontiguousarray(ctx[0].T)
    for l in range(2):
        last = l == 1
        modL, modC = mod[l, 0], mod[l, 1]
        xext = ext_from_fm(xfm)
        r1 = _run(build_L1(last), [{
            "xT": xext[j], "xcT": xcfm,
            "vec": make_vec(j, g_mix[l], modL[0], modL[1], modC[0], modC[1], conv_w[l]),
            "w_in": w_in[l], "ropec": np.ascontiguousarray(cosp[:, j * TL:j * TL + NEXT]),
            "ropes": np.ascontiguousarray(sinp[:, j * TL:j * TL + NEXT]), "perm": perm, "dft": dft} for j in R])
        del xext
        A = np.concatenate([r1[j]["AB"][0] for j in R], axis=0)
        B = np.concatenate([r1[j]["AB"][1] for j in R], axis=0)
        r2 = _run(nc2, [{"Ain": np.ascontiguousarray(A[:, j * 128:(j + 1) * 128]),
                         "Bin": np.ascontiguousarray(B[:, j * 128:(j + 1) * 128]), "W": Wf, "tw": twf} for j in R])
        four = np.concatenate([r2[j]["yT"] for j in R], axis=0)
        del A, B, r2
        wr, br, bgu, bdnT = moe_small_inputs(w_router[l], b_router[l], b_gu[l], b_dn[l])
        vec2 = make_vec2(modL, modC, g_ffn[l], g_final, attn_sink[l])
        wgu_l, wdn_l = expert_layout(w_gu[l], w_dn[l])
        maps = []
        for j in R:
            m = {"qT": r1[j]["qT"], "kT": r1[j]["kT"], "v": r1[j]["v"], "kcT": r1[j]["kcT"], "vc": r1[j]["vc"],
                 "convT": r1[j]["convT"], "fourT": np.ascontiguousarray(four[:, j * TL:(j + 1) * TL]), "xT": xfm[j],
                 "vec2": vec2, "masks": make_masks(j), "w_out": w_out[l], "wr": wr, "br": br, "w_gu": wgu_l,
                 "bgu": bgu, "w_dn": wdn_l, "bdnT": bdnT, "ident": ident}
            if not last:
                m.update({"qcT": r1[j]["qcT"], "convcT": r1[j]["convcT"], "fourcT": r1[j]["fourcT"], "xcT": xcfm})
            maps.append(m)
        r3 = _run(build_L3(last), maps)
        del maps, r1, four
        xfm = [r3[j]["xoT"] for j in R]
        if not last:
            xcfm = r3[0]["xocT"]
        del r3
    out = np.concatenate([xfm[j].T for j in R], axis=0)[None]
    return np.ascontiguousarray(out.astype(np.float32))
```
